# Optimizing a Trainium2 kernel written in Bass

```python
import jax, jax.numpy as jnp
from jax import lax
import numpy as np

D_MODEL = 2048
BATCH = 4
SEQ = 4096
DEPTH = 1

CHUNK = 64
N_META = 16
EPS = 1e-6
RG_WIDTH = 2048
RG_HEADS = 16
RG_HEAD_DIM = RG_WIDTH // RG_HEADS
CONV_WIDTH = 4
RG_C = 8.0
HG_HEADS = 16
HG_EXPAND = 128
HG_HEAD_DIM = 128
HG_WIDTH = HG_HEADS * HG_EXPAND
PEER_HEADS = 8
N_KEYS = 128
N_EXPERTS = N_KEYS * N_KEYS
PEER_TOPK = 16
D_QUERY = 256
PEER_BLOCK = 128

IN_SPLITS = (RG_WIDTH, 2 * RG_WIDTH, 2 * RG_WIDTH + HG_WIDTH, 2 * RG_WIDTH + 2 * HG_WIDTH,
             2 * RG_WIDTH + 3 * HG_WIDTH, 2 * RG_WIDTH + 4 * HG_WIDTH,
             2 * RG_WIDTH + 4 * HG_WIDTH + D_MODEL)
IN_WIDTH = 2 * RG_WIDTH + 4 * HG_WIDTH + 2 * D_MODEL

kernel_name = "hybrid_rglru_hgrn2_peer_block"


def rms_norm(x, g):
    xf = x.astype(jnp.float32)
    y = xf * lax.rsqrt(jnp.mean(xf * xf, axis=-1, keepdims=True) + EPS)
    return (y * g.astype(jnp.float32)).astype(x.dtype)


def causal_dwconv(x, w, b):
    T = x.shape[1]
    xp = jnp.pad(x, ((0, 0), (CONV_WIDTH - 1, 0), (0, 0)))
    y = b
    for k in range(CONV_WIDTH):
        y = y + xp[:, k:k + T] * w[k]
    return y


def rg_lru(x, wa, ba, wx, bx, lam):
    B, T, _ = x.shape
    xf = x.astype(jnp.float32)
    xh = xf.reshape(B, T, RG_HEADS, RG_HEAD_DIM)
    r = jax.nn.sigmoid(jnp.einsum('bthi,hij->bthj', xh, wa.astype(jnp.float32)).reshape(B, T, RG_WIDTH) + ba)
    i = jax.nn.sigmoid(jnp.einsum('bthi,hij->bthj', xh, wx.astype(jnp.float32)).reshape(B, T, RG_WIDTH) + bx)
    log_a = -RG_C * r * jax.nn.softplus(-lam.astype(jnp.float32))
    a = jnp.exp(log_a)
    u = jnp.sqrt(-jnp.expm1(2.0 * log_a)) * (i * xf)

    def combine(e, l):
        return l[0] * e[0], l[0] * e[1] + l[1]

    _, h = lax.associative_scan(combine, (a, u), axis=1)
    return h.astype(x.dtype)


def hgrn2(q, f_logits, v, og, lb, norm_g):
    B, T, _ = q.shape
    dt = q.dtype
    f = lb + (1.0 - lb) * jax.nn.sigmoid(f_logits.astype(jnp.float32))
    log_f = jnp.log(f)
    k = 1.0 - f
    qs = jax.nn.silu(q.astype(jnp.float32))
    vf = v.astype(jnp.float32)
    pad = (-T) % CHUNK
    Tp = T + pad
    nc = Tp // CHUNK

    def to_chunks(t, d):
        t = jnp.pad(t, ((0, 0), (pad, 0), (0, 0)))
        return t.reshape(B, nc, CHUNK, HG_HEADS, d).transpose(1, 0, 3, 2, 4)

    qc, kc, lfc = to_chunks(qs, HG_EXPAND), to_chunks(k, HG_EXPAND), to_chunks(log_f, HG_EXPAND)
    vc = to_chunks(vf, HG_HEAD_DIM)
    causal = jnp.tril(jnp.ones((CHUNK, CHUNK), dtype=bool))

    def step(S, inp):
        qt, kt, vt, lf = inp
        bcum = jnp.cumsum(lf, axis=2)
        diff = bcum[:, :, :, None, :] - bcum[:, :, None, :, :]
        decay = jnp.exp(jnp.where(causal[:, :, None], diff, -jnp.inf))
        scores = jnp.einsum('bhtk,bhtsk,bhsk->bhts', qt, decay, kt)
        o = (jnp.einsum('bhts,bhsv->bhtv', scores, vt)
             + jnp.einsum('bhtk,bhkv->bhtv', qt * jnp.exp(bcum), S))
        blast = bcum[:, :, -1:, :]
        S = (jnp.exp(blast[:, :, 0, :])[..., None] * S
             + jnp.einsum('bhsk,bhsv->bhkv', kt * jnp.exp(blast - bcum), vt))
        return S, o

    S0 = jnp.zeros((B, HG_HEADS, HG_EXPAND, HG_HEAD_DIM), jnp.float32)
    _, o = lax.scan(step, S0, (qc, kc, vc, lfc))
    o = o.transpose(1, 0, 3, 2, 4).reshape(B, Tp, HG_HEADS, HG_HEAD_DIM)[:, pad:]
    o = o * lax.rsqrt(jnp.mean(o * o, axis=-1, keepdims=True) + EPS)
    o = o.reshape(B, T, HG_HEADS * HG_HEAD_DIM) * norm_g.astype(jnp.float32)
    return (o * jax.nn.silu(og.astype(jnp.float32))).astype(dt)


def peer(x, wq, keys, u, v):
    B, T, D = x.shape
    n = B * T
    xt = x.reshape(n, D)
    q = (xt @ wq).reshape(n, PEER_HEADS, 2, D_QUERY // 2)
    s = jnp.einsum('nhpd,hpkd->nhpk', q, keys).astype(jnp.float32)
    s_top, i_top = lax.top_k(s, PEER_TOPK)
    cand = (s_top[:, :, 0, :, None] + s_top[:, :, 1, None, :]).reshape(n, PEER_HEADS, PEER_TOPK * PEER_TOPK)
    cand_idx = (i_top[:, :, 0, :, None] * N_KEYS + i_top[:, :, 1, None, :]).reshape(n, PEER_HEADS, PEER_TOPK * PEER_TOPK)
    best, pos = lax.top_k(cand, PEER_TOPK)
    idx = jnp.take_along_axis(cand_idx, pos, axis=-1).reshape(n, PEER_HEADS * PEER_TOPK)
    gate = jax.nn.softmax(best, axis=-1).reshape(n, PEER_HEADS * PEER_TOPK).astype(x.dtype)
    pad_n = (-n) % PEER_BLOCK
    nb = (n + pad_n) // PEER_BLOCK
    xb = jnp.pad(xt, ((0, pad_n), (0, 0))).reshape(nb, PEER_BLOCK, D)
    ib = jnp.pad(idx, ((0, pad_n), (0, 0))).reshape(nb, PEER_BLOCK, -1)
    gb = jnp.pad(gate, ((0, pad_n), (0, 0))).reshape(nb, PEER_BLOCK, -1)

    def block(args):
        xs, ids, gs = args
        ue = jnp.take(u, ids, axis=0)
        act = jax.nn.gelu(jnp.einsum('nd,ned->ne', xs, ue), approximate=False)
        ve = jnp.take(v, ids, axis=0)
        return jnp.einsum('ne,ned->nd', gs * act, ve)

    out = lax.map(block, (xb, ib, gb)).reshape(nb * PEER_BLOCK, D)[:n]
    return out.reshape(B, T, D)


def setup_inputs(seed: int = 0) -> dict:
    key = jax.random.key(seed)
    ks = jax.random.split(key, 24)
    f32 = jnp.float32
    L = DEPTH

    def nrm(k, shape, scale):
        return jax.random.normal(k, shape, f32) * scale

    a_c = jax.random.uniform(ks[11], (L, RG_WIDTH), f32, 0.9, 0.999)
    a = a_c ** (1.0 / RG_C)
    rg_lambda = jnp.log(a) - jnp.log1p(-a)
    return {
        'x': nrm(ks[0], (BATCH, SEQ, D_MODEL), 1.0),
        'meta': nrm(ks[1], (N_META, D_MODEL), 1.0),
        'ln1_g': 1.0 + nrm(ks[2], (L, D_MODEL), 0.02),
        'w_in': nrm(ks[3], (L, D_MODEL, IN_WIDTH), D_MODEL ** -0.5),
        'conv_w': nrm(ks[4], (L, CONV_WIDTH, RG_WIDTH), CONV_WIDTH ** -0.5),
        'conv_b': nrm(ks[5], (L, RG_WIDTH), 0.02),
        'rg_wa': nrm(ks[6], (L, RG_HEADS, RG_HEAD_DIM, RG_HEAD_DIM), RG_HEAD_DIM ** -0.5),
        'rg_ba': nrm(ks[7], (L, RG_WIDTH), 0.02),
        'rg_wx': nrm(ks[8], (L, RG_HEADS, RG_HEAD_DIM, RG_HEAD_DIM), RG_HEAD_DIM ** -0.5),
        'rg_bx': nrm(ks[9], (L, RG_WIDTH), 0.02),
        'rg_lambda': rg_lambda,
        'hg_lb_logits': nrm(ks[10], (L + 1, HG_WIDTH), 0.5),
        'hg_norm_g': 1.0 + nrm(ks[12], (L, HG_WIDTH), 0.02),
        'w_pa': nrm(ks[13], (L, RG_WIDTH, D_MODEL), RG_WIDTH ** -0.5),
        'w_pb': nrm(ks[14], (L, HG_WIDTH, D_MODEL), HG_WIDTH ** -0.5),
        'w_out': nrm(ks[15], (L, D_MODEL, D_MODEL), D_MODEL ** -0.5),
        'ln2_g': 1.0 + nrm(ks[16], (L, D_MODEL), 0.02),
        'peer_wq': nrm(ks[17], (L, D_MODEL, PEER_HEADS * D_QUERY), D_MODEL ** -0.5),
        'peer_keys': nrm(ks[18], (L, PEER_HEADS, 2, N_KEYS, D_QUERY // 2), (D_QUERY // 2) ** -0.5),
        'peer_u': nrm(ks[19], (L, N_EXPERTS, D_MODEL), D_MODEL ** -0.5),
        'peer_v': nrm(ks[20], (L, N_EXPERTS, D_MODEL), PEER_HEADS ** -0.5),
        'final_g': 1.0 + nrm(ks[21], (D_MODEL,), 0.02),
    }


def reference(x, meta, ln1_g, w_in, conv_w, conv_b, rg_wa, rg_ba, rg_wx, rg_bx, rg_lambda,
              hg_lb_logits, hg_norm_g, w_pa, w_pb, w_out, ln2_g, peer_wq, peer_keys,
              peer_u, peer_v, final_g):
    B = x.shape[0]
    h = jnp.concatenate([jnp.broadcast_to(meta[None].astype(x.dtype), (B, N_META, D_MODEL)), x], axis=1)
    lb_all = jnp.cumsum(jax.nn.softmax(hg_lb_logits.astype(jnp.float32), axis=0), axis=0)
    for l in range(DEPTH):
        xn = rms_norm(h, ln1_g[l])
        proj = xn @ w_in[l]
        xa, ya, qb, fb, ib, gb, za, zb = jnp.split(proj, IN_SPLITS, axis=-1)
        ha = rg_lru(causal_dwconv(xa, conv_w[l], conv_b[l]), rg_wa[l], rg_ba[l], rg_wx[l], rg_bx[l], rg_lambda[l])
        y_a = jax.nn.gelu(ya, approximate=False) * ha
        y_b = hgrn2(qb, fb, ib, gb, lb_all[l], hg_norm_g[l])
        mixed = jax.nn.sigmoid(za) * (y_a @ w_pa[l]) + jax.nn.sigmoid(zb) * (y_b @ w_pb[l])
        h = h + mixed @ w_out[l]
        h = h + peer(rms_norm(h, ln2_g[l]), peer_wq[l], peer_keys[l], peer_u[l], peer_v[l])
    h = rms_norm(h, final_g)
    return h[:, N_META:]
```

```python
import numpy as np
from contextlib import ExitStack
import concourse.bass as bass
import concourse.mybir as mybir
from concourse.bass_utils import run_bass_kernel_spmd

F32 = mybir.dt.float32
BF16 = mybir.dt.bfloat16
U32 = mybir.dt.uint32
I32 = mybir.dt.int32
AF = mybir.ActivationFunctionType
ALU = mybir.AluOpType
AX = mybir.AxisListType

D = 2048
NCH = 16
TB = 512
CH = 64
EPS = 1e-6
NKEY = 128
NEXP = 16384

PP_G1, PP_G2, PP_GF = 0, 16, 32
PP_CW = 48
PP_CB = 112
PP_BA = 128
PP_BX = 144
PP_LAM = 160
PP_L0 = 176
PP_L1 = 192
PP_NG = 208
NPP = 224
C_ID = 0
C_MASKT = 128
C_RESET = 192
C_IOTA = 704
C_ONES = 832
C_EPS = 960
C_ONE = 961
C_IOTA16 = 962
C_THR16 = 978
NCONST = 994


class Buf:
    def __init__(self, name):
        self.name = name
        self.w = {}
        self.r = {}

    def _keys(self, k):
        if k == '*':
            return list(set(self.w) | set(self.r) | {'*'})
        return [k, '*']

    def rdeps(self, k):
        return [self.w[kk] for kk in self._keys(k) if kk in self.w]

    def wdeps(self, k):
        waw = [self.w[kk] for kk in self._keys(k) if kk in self.w]
        war = []
        for kk in self._keys(k):
            for sk, v in self.r.get(kk, {}).items():
                war.append((sk, v))
        return waw, war

    def did_read(self, k, ev):
        d = self.r.setdefault(k, {})
        d[ev[0]] = max(d.get(ev[0], 0), ev[1])

    def did_write(self, k, ev):
        if k == '*':
            self.w = {'*': ev}
            self.r = {}
        else:
            self.w[k] = ev
            self.r[k] = {}


def _norm(lst):
    out = []
    for x in lst:
        if isinstance(x, Buf):
            out.append((x, '*'))
        elif isinstance(x, list):
            out.extend(_norm(x))
        else:
            out.append(x)
    return out


class Sched:
    EPOCH = 30000
    DMA_EPOCH = 1800

    def __init__(self, nc, stack):
        self.nc = nc
        self.stack = stack
        self.eng = {'pe': nc.tensor, 'act': nc.scalar, 'dve': nc.vector, 'pool': nc.gpsimd, 'sp': nc.sync}
        self.prog = {k: [] for k in self.eng}
        self.sems = {}
        self.cnt = {}
        self.epoch = {}
        self.waited = {k: {} for k in self.eng}
        self.nsem = 0

    def _sem(self, key):
        if key not in self.sems:
            self.nsem += 1
            self.sems[key] = self.stack.enter_context(self.nc.semaphore("s%d" % self.nsem))
            self.cnt[key] = 0
        return self.sems[key]

    def _next_event(self, base, inc, limit):
        ep = self.epoch.get(base, 0)
        key = (base, ep)
        self._sem(key)
        if self.cnt[key] + inc > limit:
            ep += 1
            self.epoch[base] = ep
            key = (base, ep)
            self._sem(key)
        self.cnt[key] += inc
        return (key, self.cnt[key])

    def _waits(self, eng, reads, writes):
        deps = []
        for b, k in reads:
            for ev in b.rdeps(k):
                deps.append((ev, 'raw'))
        for b, k in writes:
            waw, war = b.wdeps(k)
            for ev in waw:
                deps.append((ev, 'waw'))
            for ev in war:
                deps.append((ev, 'war'))
        waits = []
        wd = self.waited[eng]
        for (sk, v), kind in deps:
            base = sk[0]
            if base == ('eng', eng):
                if eng == 'pe' or kind != 'raw':
                    continue
            if wd.get(sk, 0) >= v:
                continue
            wd[sk] = v
            waits.append((sk, v))
        return waits

    def op(self, eng, fn, reads=(), writes=()):
        reads = _norm(reads)
        writes = _norm(writes)
        waits = self._waits(eng, reads, writes)
        ev = self._next_event(('eng', eng), 1, self.EPOCH)
        self.prog[eng].append((waits, fn, ev[0], 1))
        for b, k in reads:
            b.did_read(k, ev)
        for b, k in writes:
            b.did_write(k, ev)
        return ev

    def dma(self, q, out, in_, reads=(), writes=(), chan=None):
        reads = _norm(reads)
        writes = _norm(writes)
        waits = self._waits(q, reads, writes)
        ev = self._next_event(('dma', chan), 16, self.DMA_EPOCH * 16)
        self.prog[q].append((waits, lambda e: e.dma_start(out=out, in_=in_), ev[0], 16))
        for b, k in reads:
            b.did_read(k, ev)
        for b, k in writes:
            b.did_write(k, ev)
        return ev

    def final_wait(self, eng, events):
        waits = []
        for sk, v in events:
            waits.append((sk, v))
        self.prog[eng].append((waits, None, None, 0))

    def emit(self):
        nc = self.nc
        with nc.Block() as block:
            decs = {'pe': block.tensor, 'act': block.scalar, 'dve': block.vector,
                    'pool': block.gpsimd, 'sp': block.sync}
            for name in ['sp', 'pool', 'pe', 'act', 'dve']:
                lst = self.prog[name]
                if not lst:
                    continue

                def body(e, lst=lst):
                    for waits, fn, semkey, inc in lst:
                        for sk, v in waits:
                            e.wait_ge(self.sems[sk], v)
                        if fn is not None:
                            r = fn(e)
                            r.then_inc(self.sems[semkey], inc)
                decs[name](body)


def build_program(pre_blocks, main_blocks, do_peer=True, dbg=False):
    nc = bass.Bass("TRN2", target_bir_lowering=False)
    npre = sum(pre_blocks)
    nmain = sum(main_blocks)

    def din(name, shape, dt=F32):
        return nc.dram_tensor(name, list(shape), dt, kind="ExternalInput").ap()

    xpre = din("xpre", [max(npre, 128), D])
    xmain = din("xmain", [nmain, D])
    maskpre = din("maskpre", [128, max(npre, 128)])
    w_in = din("w_in", [128, 128, D])
    w_pa = din("w_pa", [16, 128, D])
    w_pb = din("w_pb", [16, 128, D])
    w_out = din("w_out", [16, 128, D])
    wq = din("wq", [16, 128, D])
    rg_wa = din("rg_wa", [16, 128, 128])
    rg_wx = din("rg_wx", [16, 128, 128])
    pp_d = din("pp", [128, NPP])
    consts_d = din("consts", [128, NCONST])
    keysT = din("keysT", [16, 128, 128])
    uT = din("uT", [128, 128, D])
    vP = din("vP", [NEXP, D])
    y = nc.dram_tensor("y", [nmain, D], F32, kind="ExternalOutput").ap()
    wd_scr = nc.dram_tensor("wd_scr", [4, 128, 128 * 128], BF16, kind="Internal").ap()

    stack = ExitStack()
    with stack:
        S = Sched(nc, stack)
        bufs = {}

        def sb(name, shape, dt=F32):
            t = stack.enter_context(nc.sbuf_tensor("s_" + name, list(shape), dt))
            b = Buf(name)
            bufs[name] = b
            return t, b

        def psb(name, shape, dt=F32):
            t = stack.enter_context(nc.psum_tensor("p_" + name, list(shape), dt))
            b = Buf(name)
            return t, b

        B_y = Buf("y")
        B_wd = Buf("wd")

        HT, B_HT = sb("HT", [128, NCH, TB], F32)
        XT, B_XT = sb("XT", [128, NCH, TB], BF16)
        AR1, B_YA = sb("AR1", [128, NCH * TB], BF16)
        AR2, B_YB = sb("AR2", [128, NCH * TB], BF16)
        AR3, B_A3 = sb("AR3", [128, 2 * NCH * TB], BF16)
        YA = AR1[:, :].rearrange("p (c t) -> p c t", t=TB)
        YB = AR2[:, :].rearrange("p (c t) -> p c t", t=TB)
        MIX = AR3[:, 0:NCH * TB].rearrange("p (c t) -> p c t", t=TB)
        B_MIX = B_A3
        _xs = AR3[:, NCH * TB:2 * NCH * TB].bitcast(F32)
        xst = [(_xs[:, 0:D], (B_A3, 'x0')), (_xs[:, D:2 * D], (B_A3, 'x1'))]
        XS_ALIAS = {0: [(B_A3, ('hg', i)) for i in (0, 1, 2, 3)], 1: [(B_A3, ('hg', i)) for i in (3, 4, 5, 6)]}
        NSLAB = 6
        slabs = [sb("slab%d" % i, [128, NCH, 128], BF16) for i in range(NSLAB)]
        pp, B_pp = sb("pp", [128, NPP], F32)
        cst, B_cst = sb("cst", [128, NCONST], F32)
        identb, B_identb = sb("identb", [128, 128], BF16)
        maskTb, B_maskTb = sb("maskTb", [64, 64], F32)
        drv, B_drv = sb("drv", [128, 96], F32)
        wab, B_wab = sb("wab", [128, 2, 2, 128], BF16)
        xa_tail, B_xat = sb("xa_tail", [128, 16, 4], F32)
        hcar, B_hcar = sb("hcar", [128, 16], F32)
        Sst, B_S = sb("Sst", [128, 16, 128], F32)
        Sall, B_Sall = sb("Sall", [128, 8, 128], F32)
        Sbf8, B_Sbf8 = sb("Sbf8", [128, 8, 128], BF16)
        mpre, B_mpre = sb("mpre", [128, TB], F32)
        rstd, B_rstd = sb("rstd", [128, TB], F32)
        NW = 12
        WKW = TB + 4
        WK, _ = sb("WK", [128, NW * WKW], F32)
        wk = [(WK[:, i * WKW:(i + 1) * WKW], Buf("wk%d" % i)) for i in range(NW)]
        NWB = 6
        wkb = [sb("wkb%d" % i, [128, TB], BF16) for i in range(NWB)]
        vbf, B_vbf = sb("vbf", [64, 8, 128], BF16)
        scm8, B_scm8 = sb("scm8", [64, 8, 64], BF16)
        ket8, B_ket8 = sb("ket8", [64, 8, 128], BF16)
        sml, B_sml = sb("sml", [128, 64], F32)
        tv, B_tv = sb("tv", [128, 256], F32)
        tix, B_tix = sb("tix", [128, 256], U32)
        tif, B_tif = sb("tif", [128, 256], F32)
        bv, B_bv = sb("bv", [128, 128], F32)
        bpos, B_bpos = sb("bpos", [128, 128], U32)
        pk, B_pk = sb("pk", [128, 6, 128], F32)
        hs8, B_hs8 = sb("hs8", [128, 16], F32)
        ijg, B_ijg = sb("ijg", [128, 4, 3, 128], BF16)

        pbanks = [psb("pb%d" % i, [128, 512], F32) for i in range(7)]
        pbb, B_pbb = psb("pbb", [128, 1024], BF16)
        rot = [0]

        def ps():
            i = 1 + (rot[0] % 6)
            rot[0] += 1
            return pbanks[i]

        wrot = [0]

        def wtile():
            i = wrot[0] % NW
            wrot[0] += 1
            return wk[i]

        wbrot = [0]

        def wbtile():
            i = wbrot[0] % NWB
            wbrot[0] += 1
            return wkb[i]

        srot = {'base': 0, 'heads': 0, 'pre': 0}
        slab_mode = ['base']
        ex_mix = [(AR3[:, j * 2048:(j + 1) * 2048].rearrange("p (c n) -> p c n", n=128),
                   [(B_A3, 4 * j + i) for i in range(4)]) for j in range(4)]
        ex_ya = [(AR1[:, j * 2048:(j + 1) * 2048].rearrange("p (c n) -> p c n", n=128),
                  [(B_YA, 4 * j + i) for i in range(4)]) for j in range(4)]
        ex_yb = [(AR2[:, j * 2048:(j + 1) * 2048].rearrange("p (c n) -> p c n", n=128),
                  [(B_YB, 4 * j + i) for i in range(4)]) for j in range(4)]
        rings = {'base': [(t, [b]) for (t, b) in slabs],
                 'heads': [(t, [b]) for (t, b) in slabs] + ex_mix,
                 'pre': [(t, [b]) for (t, b) in slabs] + ex_mix + ([] if PRE_PAIR else ex_ya + ex_yb)}

        def load_slab(src_ap):
            mode = slab_mode[0]
            ring = rings[mode]
            i = srot[mode] % len(ring)
            srot[mode] += 1
            t, b = ring[i]
            S.dma('pool', t[:, :, :], src_ap, reads=[], writes=b, chan=('slab', i))
            return t, b

        def wcols(w, c0):
            return w[c0 // 128, :, :].rearrange("p (c n) -> p c n", n=128)

        act_log = []

        def act(out, in_, func, reads, writes, bias=None, scale=None):
            act_log.append(str(func))
            kw = {}
            if bias is not None:
                kw['bias'] = bias
            if scale is not None:
                kw['scale'] = scale
            S.op('act', lambda e: e.activation(out=out, in_=in_, func=func, **kw), reads, writes)

        def tt(out, a, b, op, reads, writes, eng='dve'):
            S.op(eng, lambda e: e.tensor_tensor(out=out, in0=a, in1=b, op=op), reads, writes)

        def ts(out, a, s1, s2, op0, op1, reads, writes, eng='dve'):
            if op1 is None:
                S.op(eng, lambda e: e.tensor_scalar(out=out, in0=a, scalar1=s1, scalar2=None, op0=op0), reads, writes)
            else:
                S.op(eng, lambda e: e.tensor_scalar(out=out, in0=a, scalar1=s1, scalar2=s2, op0=op0, op1=op1), reads, writes)

        def stt(out, a, s, b, op0, op1, reads, writes):
            S.op('dve', lambda e: e.scalar_tensor_tensor(out=out, in0=a, scalar=s, in1=b, op0=op0, op1=op1), reads, writes)

        def cp(eng, out, in_, reads, writes):
            if eng == 'act':
                S.op('act', lambda e: e.copy(out=out, in_=in_), reads, writes)
            else:
                S.op(eng, lambda e: e.tensor_copy(out=out, in_=in_), reads, writes)

        def mm(out, lhsT, rhs, start, stop, reads, writes):
            S.op('pe', lambda e: e.matmul(out, lhsT, rhs, start=start, stop=stop), reads, writes)

        def tr(out, in_, ident, reads, writes):
            S.op('pe', lambda e: e.transpose(out, in_, ident), reads, writes)

        S.dma('sp', pp[:, :], pp_d[:, :], writes=[B_pp], chan='pp')
        S.dma('sp', cst[:, :], consts_d[:, :], writes=[B_cst], chan='cst')
        cp('dve', identb[:, :], cst[:, C_ID:C_ID + 128], [B_cst], [B_identb])
        cp('dve', maskTb[:, :], cst[0:64, C_MASKT:C_MASKT + 64], [B_cst], [B_maskTb])
        ident = cst[:, C_ID:C_ID + 128]
        ones_f = cst[:, C_ONES:C_ONES + 128]
        eps_c = cst[:, C_EPS:C_EPS + 1]
        one_c = cst[:, C_ONE:C_ONE + 1]
        act(drv[:, 48:64], pp[:, PP_LAM:PP_LAM + 16], AF.Exp, [B_pp], [(B_drv, 't')], scale=-1.0)
        act(drv[:, 48:64], drv[:, 48:64], AF.Ln, [(B_drv, 't'), B_cst], [(B_drv, 't')], bias=one_c, scale=1.0)
        ts(drv[:, 0:16], drv[:, 48:64], -8.0, None, ALU.mult, None, [(B_drv, 't')], [(B_drv, 'c1')])
        tt(drv[:, 48:64], pp[:, PP_L0:PP_L0 + 16], pp[:, PP_L1:PP_L1 + 16], ALU.subtract, [B_pp, (B_drv, 'c1')], [(B_drv, 't')])
        act(drv[:, 16:32], drv[:, 48:64], AF.Sigmoid, [(B_drv, 't')], [(B_drv, 'lb')])
        ts(drv[:, 32:48], drv[:, 16:32], -1.0, 1.0, ALU.mult, ALU.add, [(B_drv, 'lb')], [(B_drv, 'oml')])
        ts(drv[:, 48:64], drv[:, 32:48], -1.0, None, ALU.mult, None, [(B_drv, 'oml'), (B_drv, 't')], [(B_drv, 't')])
        ts(drv[:, 64:80], drv[:, 0:16], 2.0, None, ALU.mult, None, [(B_drv, 'c1')], [(B_drv, 'c2')])
        S.op('dve', lambda e: e.memset(xa_tail[:, :, :], 0.0), [], [B_xat])
        S.op('dve', lambda e: e.memset(hcar[:, :], 0.0), [], [B_hcar])
        S.op('dve', lambda e: e.memset(Sst[:, :, :], 0.0), [], [B_S])
        B_drvall = B_drv

        def rms_rows(nt, gcol, out_is_xt=True):
            pt, pbuf = ps()
            for c in range(NCH):
                w, bw = wtile()
                act(w[:, :nt], HT[:, c, :nt], AF.Square, [(B_HT, c)], [bw])
                mm(pt[:, :nt], ones_f, w[:, :nt], c == 0, c == NCH - 1, [bw, B_cst], [pbuf])
            act(rstd[:, :nt], pt[:, :nt], AF.Ln, [pbuf, B_cst], [B_rstd], bias=eps_c, scale=1.0 / D)
            act(rstd[:, :nt], rstd[:, :nt], AF.Exp, [B_rstd], [B_rstd], scale=-0.5)
            if out_is_xt:
                for c in range(NCH):
                    stt(XT[:, c, :nt], HT[:, c, :nt], pp[:, gcol + c:gcol + c + 1], rstd[:, :nt],
                        ALU.mult, ALU.mult, [(B_HT, c), B_pp, B_rstd], [(B_XT, c)])

        def load_block(xsrc, t0, nt):
            for ti in range(nt // 128):
                st, bst = xst[ti % 2]
                S.dma('sp', st[:, :], xsrc[t0 + ti * 128:t0 + (ti + 1) * 128, :], writes=[bst] + XS_ALIAS[ti % 2], chan=('xst', ti % 2))
                for c4 in range(4):
                    pt, pbuf = ps()
                    for j in range(4):
                        c = c4 * 4 + j
                        tr(pt[:, j * 128:(j + 1) * 128], st[:, c * 128:(c + 1) * 128], ident, [bst, B_cst], [pbuf])
                    cp('act', HT[:, c4 * 4:(c4 + 1) * 4, ti * 128:(ti + 1) * 128],
                       pt[:, :].rearrange("p (j t) -> p j t", j=4), [pbuf],
                       [(B_HT, c4 * 4 + j) for j in range(4)])

        def proj_fm(slab, bslab, nt):
            pt, pbuf = ps()
            for c in range(NCH):
                mm(pt[:, :nt], slab[:, c, :], XT[:, c, :nt], c == 0, c == NCH - 1, [bslab, (B_XT, c)], [pbuf])
            return pt, pbuf

        rg_ps_rot = [0]

        def ps_rg():
            i = 1 + (rg_ps_rot[0] % 3)
            rg_ps_rot[0] += 1
            return pbanks[i]

        hg_ps_rot = [0]

        def ps_hg():
            i = 4 + (hg_ps_rot[0] % 3)
            hg_ps_rot[0] += 1
            return pbanks[i]

        _hx = AR3[:, NCH * TB:2 * NCH * TB].bitcast(F32)
        htiles = [(_hx[:, i * WKW:(i + 1) * WKW], (B_A3, ('hg', i))) for i in range(7)]
        hrot = [0]

        def htile():
            i = hrot[0] % 7
            hrot[0] += 1
            return htiles[i]

        def _mk_rot(banks):
            st = [0]

            def f():
                i = banks[st[0] % len(banks)]
                st[0] += 1
                return pbanks[i]
            return f

        R0 = {'wt': wtile, 'xcb': wkb[0], 'kd': wkb[1], 'ke': wkb[2], 'vT': wkb[5],
              'vbf': (vbf, B_vbf), 'ket8': (ket8, B_ket8), 'Sall': (Sall, B_Sall), 'sml': (sml, B_sml),
              'ps_rg': ps_rg, 'ps_hg': ps_hg, 'one_bank': False}
        _a1 = AR1[:, :].bitcast(F32)
        r1_tiles = [(_a1[:, i * WKW:(i + 1) * WKW], Buf("r1t%d" % i)) for i in range(7)]
        r1_rot = [0]

        def r1_wt():
            i = r1_rot[0] % 7
            r1_rot[0] += 1
            return r1_tiles[i]

        r1_b16 = [(AR2[:, i * 512:(i + 1) * 512], Buf("r1b%d" % i)) for i in range(4)]
        r1_vbf = (AR2[0:64, 2048:3072].rearrange("p (c v) -> p c v", v=128), Buf("r1vbf"))
        r1_ket8 = (AR2[0:64, 3072:4096].rearrange("p (c v) -> p c v", v=128), Buf("r1ket8"))
        r1_Sall = (AR2[:, 4096:6144].bitcast(F32).rearrange("p (c v) -> p c v", v=128), Buf("r1Sall"))
        r1_sml = (AR2[:, 6144:6272].bitcast(F32), Buf("r1sml"))
        R1 = {'wt': r1_wt, 'xcb': r1_b16[0], 'kd': r1_b16[1], 'ke': r1_b16[2], 'vT': r1_b16[3],
              'vbf': r1_vbf, 'ket8': r1_ket8, 'Sall': r1_Sall, 'sml': r1_sml,
              'ps_rg': _mk_rot([3, 4]), 'ps_hg': _mk_rot([0]), 'one_bank': True}
        R0pre = dict(R0)
        R0pre.update({'ps_rg': _mk_rot([1, 2]), 'ps_hg': _mk_rot([5, 6]), 'one_bank': True})
        R1_ALLBUFS = [b for _, b in r1_tiles] + [b for _, b in r1_b16] + [r1_vbf[1], r1_ket8[1], r1_Sall[1], r1_sml[1]]

        B_dum = Buf("dummy_bank")

        def proj_fm2(slab, bslab, nt, psf):
            pt, pbuf = psf()
            for c in range(NCH):
                mm(pt[:, :nt], slab[:, c, :], XT[:, c, :nt], c == 0, c == NCH - 1, [bslab, (B_XT, c)], [pbuf])
            if WARM_K and slab_mode[0] == 'pre' and not PRE_PAIR:
                for _ in range(WARM_K):
                    mm(pbanks[0][0][:, :nt], identb[:, :], XT[:, 0, :nt], True, True, [B_identb, (B_XT, 0)], [B_dum])
            return pt, pbuf

        def rglru_head(g, nt, main, t0, R=None):
            R = R or R0
            sx, bsx = load_slab(wcols(w_in, g * 128))
            px, bpx = proj_fm2(sx, bsx, nt, R['ps_rg'])
            xe, bxe = R['wt']()
            cp('dve', xe[:, 0:3], xa_tail[:, g, 0:3], [B_xat], [bxe])
            cp('act', xe[:, 3:3 + nt], px[:, :nt], [bpx], [bxe])
            cp('dve', xa_tail[:, g, 0:3], xe[:, nt:nt + 3], [bxe], [B_xat])
            yield
            xc, bxc = R['wt']()
            cw = lambda k: pp[:, PP_CW + g * 4 + k:PP_CW + g * 4 + k + 1]
            ts(xc[:, :nt], xe[:, 3:3 + nt], cw(3), pp[:, PP_CB + g:PP_CB + g + 1], ALU.mult, ALU.add, [bxe, B_pp], [bxc])
            yield
            xcb, bxcb = R['xcb']
            for k in range(3):
                if k < 2:
                    stt(xc[:, :nt], xe[:, k:k + nt], cw(k), xc[:, :nt], ALU.mult, ALU.add, [bxe, B_pp, bxc], [bxc])
                else:
                    stt(xcb[:, :nt], xe[:, k:k + nt], cw(k), xc[:, :nt], ALU.mult, ALU.add, [bxe, B_pp, bxc], [bxcb])
                yield
            wsl = g % 2
            S.dma('pool', wab[:, wsl, 0, :], rg_wa[g, :, :], writes=[(B_wab, wsl)], chan=('wab', wsl))
            S.dma('pool', wab[:, wsl, 1, :], rg_wx[g, :, :], writes=[(B_wab, wsl)], chan=('wab', wsl))
            yield
            pr, bpr = R['ps_rg']()
            mm(pr[:, :nt], wab[:, wsl, 0, :], xcb[:, :nt], True, True, [(B_wab, wsl), bxcb], [bpr])
            pi, bpi = R['ps_rg']()
            mm(pi[:, :nt], wab[:, wsl, 1, :], xcb[:, :nt], True, True, [(B_wab, wsl), bxcb], [bpi])
            yield
            r, br = R['wt']()
            act(r[:, :nt], pr[:, :nt], AF.Sigmoid, [bpr, B_pp], [br], bias=pp[:, PP_BA + g:PP_BA + g + 1], scale=1.0)
            ig, big = R['wt']()
            act(ig[:, :nt], pi[:, :nt], AF.Sigmoid, [bpi, B_pp], [big], bias=pp[:, PP_BX + g:PP_BX + g + 1], scale=1.0)
            yield
            a, ba_ = R['wt']()
            act(a[:, :nt], r[:, :nt], AF.Exp, [br, B_drv], [ba_], scale=drv[:, g:g + 1])
            s2, bs2 = R['wt']()
            act(s2[:, :nt], r[:, :nt], AF.Exp, [br, B_drv], [bs2], scale=drv[:, 64 + g:65 + g])
            tt(ig[:, :nt], ig[:, :nt], xcb[:, :nt], ALU.mult, [big, bxcb], [big])
            yield
            act(s2[:, :nt], s2[:, :nt], AF.Ln, [bs2, B_cst], [bs2], bias=one_c, scale=-1.0)
            act(s2[:, :nt], s2[:, :nt], AF.Exp, [bs2], [bs2], scale=0.5)
            yield
            tt(ig[:, :nt], ig[:, :nt], s2[:, :nt], ALU.mult, [big, bs2], [big])
            if not main:
                tt(ig[:, :nt], ig[:, :nt], mpre[:, :nt], ALU.mult, [big, B_mpre], [big])
            yield
            hs, bhs = R['wt']()
            S.op('dve', lambda e: e.tensor_tensor_scan(out=hs[:, :nt], data0=a[:, :nt], data1=ig[:, :nt],
                                                        initial=hcar[:, g:g + 1], op0=ALU.mult, op1=ALU.add),
                 [ba_, big, B_hcar], [bhs])
            yield
            cp('dve', hcar[:, g:g + 1], hs[:, nt - 1:nt], [bhs], [B_hcar])
            if main:
                for _ in range(RG_GELU_DELAY):
                    yield
                sy, bsy = load_slab(wcols(w_in, 2048 + g * 128))
                py, bpy = proj_fm2(sy, bsy, nt, R['ps_rg'])
                gy, bgy = R['wt']()
                act(gy[:, :nt], py[:, :nt], AF.Gelu, [bpy], [bgy])
                yield
                tt(YA[:, g, :nt], gy[:, :nt], hs[:, :nt], ALU.mult, [bgy, bhs], [(B_YA, g)])

        def hgrn2_head(g, nt, main, R=None):
            R = R or R0
            vbf, B_vbf = R['vbf']
            ket8, B_ket8 = R['ket8']
            Sall, B_Sall = R['Sall']
            sml, B_sml = R['sml']
            nch = nt // CH
            v3 = lambda t: t[:, :nt].rearrange("p (c t) -> p c t", t=CH)
            sv, bsv = load_slab(wcols(w_in, 8192 + g * 128))
            pvf, bpvf = proj_fm2(sv, bsv, nt, R['ps_hg'])
            vT, bvT = R['vT']
            cp('act', vT[:, :nt], pvf[:, :nt], [bpvf], [bvT])
            yield
            for c in range(nch):
                tr(pbb[0:64, c * 128:(c + 1) * 128], vT[:, c * CH:(c + 1) * CH], identb[:, :], [bvT, B_identb], [B_pbb])
            cp('dve', vbf[:, 0:nch, :], pbb[0:64, 0:nch * 128].rearrange("p (c k) -> p c k", k=128), [B_pbb], [B_vbf])
            yield
            sf, bsf = load_slab(wcols(w_in, 6144 + g * 128))
            pf, bpf = proj_fm2(sf, bsf, nt, R['ps_hg'])
            f, bf_ = htile()
            act(f[:, :nt], pf[:, :nt], AF.Sigmoid, [bpf], [bf_])
            yield
            lf, blf = htile()
            act(lf[:, :nt], f[:, :nt], AF.Ln, [bf_, B_drv], [blf], bias=drv[:, 16 + g:17 + g], scale=drv[:, 32 + g:33 + g])
            yield
            ts(f[:, :nt], f[:, :nt], drv[:, 48 + g:49 + g], drv[:, 32 + g:33 + g], ALU.mult, ALU.add, [bf_, B_drv], [bf_])
            bc, bbc = htile()
            S.op('dve', lambda e: e.tensor_tensor_scan(out=bc[:, :nt], data0=cst[:, C_RESET:C_RESET + nt], data1=lf[:, :nt],
                                                        initial=0.0, op0=ALU.mult, op1=ALU.add),
                 [B_cst, blf], [bbc])
            yield
            cp('dve', sml[:, 0:nch], v3(bc)[:, :, 31], [bbc], [B_sml])
            yield
            tt(sml[:, 8:8 + nch], v3(bc)[:, :, 63], sml[:, 0:nch], ALU.subtract, [bbc, B_sml], [B_sml])
            act(sml[:, 16:16 + nch], v3(bc)[:, :, 63], AF.Exp, [bbc], [B_sml])
            yield
            act(sml[:, 8:8 + nch], sml[:, 8:8 + nch], AF.Exp, [B_sml], [B_sml])
            act(sml[:, 24:24 + nch], sml[:, 0:nch], AF.Exp, [B_sml], [B_sml])
            tt(v3(bc), v3(bc), sml[:, 0:nch].unsqueeze(2).to_broadcast([128, nch, CH]), ALU.subtract, [bbc, B_sml], [bbc])
            yield
            act(lf[:, :nt], bc[:, :nt], AF.Exp, [bbc], [blf], scale=-1.0)
            act(bc[:, :nt], bc[:, :nt], AF.Exp, [bbc], [bbc])
            yield
            kd, bkd = R['kd']
            tt(kd[:, :nt], f[:, :nt], lf[:, :nt], ALU.mult, [bf_, blf], [bkd])
            yield
            ke, bke = R['ke']
            tt(v3(ke), v3(kd), sml[:, 8:8 + nch].unsqueeze(2).to_broadcast([128, nch, CH]), ALU.mult, [bkd, B_sml], [bke])
            yield
            if main:
                sq_, bsq = load_slab(wcols(w_in, 4096 + g * 128))
                pq, bpq = proj_fm2(sq_, bsq, nt, R['ps_hg'])
                qs, bqs = htile()
                act(qs[:, :nt], pq[:, :nt], AF.Silu, [bpq], [bqs])
                yield
                qd, bqd = wkb[3]
                tt(qd[:, :nt], qs[:, :nt], bc[:, :nt], ALU.mult, [bqs, bbc], [bqd])
                yield
                qe, bqe = wkb[4]
                tt(v3(qe), v3(qd), sml[:, 24:24 + nch].unsqueeze(2).to_broadcast([128, nch, CH]), ALU.mult, [bqd, B_sml], [bqe])
                yield
                so, bso = load_slab(wcols(w_in, 10240 + g * 128))
                pog, bpog = proj_fm2(so, bso, nt, R['ps_hg'])
                sgo, bsgo = htile()
                act(sgo[:, :nt], pog[:, :nt], AF.Silu, [bpog], [bsgo])
                yield
            po, bpo = pbanks[0]
            csl = lambda c: slice(c * CH, (c + 1) * CH)
            for c in range(nch):
                tr(pbb[0:64, c * 128:(c + 1) * 128], ke[:, csl(c)], identb[:, :], [bke, B_identb], [B_pbb])
            cp('act', ket8[:, 0:nch, :], pbb[0:64, 0:nch * 128].rearrange("p (c k) -> p c k", k=128), [B_pbb], [B_ket8])
            yield
            if main:
                psc, bpsc = R['ps_hg']()
                for c in range(nch):
                    mm(psc[0:64, c * 64:(c + 1) * 64], kd[:, csl(c)], qd[:, csl(c)], True, True, [bkd, bqd], [bpsc])
                tt(scm8[:, 0:nch, :], psc[0:64, 0:nch * 64].rearrange("p (c t) -> p c t", t=64),
                   maskTb[:, :].unsqueeze(1).to_broadcast([64, nch, 64]), ALU.mult, [bpsc, B_maskTb], [B_scm8])
                cp('act', Sbf8[:, 0, :], Sst[:, g, :], [(B_S, g)], [(B_Sbf8, 0)])
                yield
            def chain_step(c, psn_ap, bpsn):
                src, bsrc = (Sst[:, g, :], (B_S, g)) if c == 0 else (Sall[:, c, :], (B_Sall, c))
                dst, bdst = (Sst[:, g, :], (B_S, g)) if c == nch - 1 else (Sall[:, c + 1, :], (B_Sall, c + 1))
                stt(dst, src, sml[:, 16 + c:17 + c], psn_ap, ALU.mult, ALU.add, [bsrc, B_sml, bpsn], [bdst])

            if R['one_bank']:
                for c4 in range((nch + 3) // 4):
                    psn, bpsn = R['ps_hg']()
                    grp_l = []
                    for j in range(min(4, nch - c4 * 4)):
                        c = c4 * 4 + j
                        mm(psn[:, j * 128:(j + 1) * 128], ket8[:, c, :], vbf[:, c, :], True, True, [B_ket8, B_vbf], [bpsn])
                        grp_l.append((c, psn[:, j * 128:(j + 1) * 128], bpsn))
                    yield
                    for c, ap_, bb in grp_l:
                        chain_step(c, ap_, bb)
                        yield
            else:
                psn_l = []
                for c4 in range((nch + 3) // 4):
                    psn, bpsn = R['ps_hg']()
                    for j in range(min(4, nch - c4 * 4)):
                        c = c4 * 4 + j
                        mm(psn[:, j * 128:(j + 1) * 128], ket8[:, c, :], vbf[:, c, :], True, True, [B_ket8, B_vbf], [bpsn])
                        psn_l.append((psn[:, j * 128:(j + 1) * 128], bpsn))
                yield
                for c in range(nch):
                    chain_step(c, psn_l[c][0], psn_l[c][1])
                    yield
            if main:
                if nch > 1:
                    cp('act', Sbf8[:, 1:nch, :], Sall[:, 1:nch, :], [(B_Sall, c) for c in range(1, nch)],
                       [(B_Sbf8, c) for c in range(1, nch)])
                yield
                for c in range(nch):
                    mm(po[:, csl(c)], vbf[:, c, :], scm8[:, c, :], True, False, [B_vbf, B_scm8], [bpo])
                    mm(po[:, csl(c)], Sbf8[:, c, :], qe[:, csl(c)], False, True, [(B_Sbf8, c), bqe], [bpo])
                yield
                osq, bosq = htile()
                act(osq[:, :nt], po[:, :nt], AF.Square, [bpo], [bosq])
                pm, bpm = R['ps_hg']()
                mm(pm[:, :nt], ones_f, osq[:, :nt], True, True, [B_cst, bosq], [bpm])
                yield
                act(osq[:, :nt], pm[:, :nt], AF.Ln, [bpm, B_cst], [bosq], bias=eps_c, scale=1.0 / 128)
                yield
                act(osq[:, :nt], osq[:, :nt], AF.Exp, [bosq], [bosq], scale=-0.5)
                yield
                tt(osq[:, :nt], po[:, :nt], osq[:, :nt], ALU.mult, [bpo, bosq], [bosq])
                yield
                stt(YB[:, g, :nt], osq[:, :nt], pp[:, PP_NG + g:PP_NG + g + 1], sgo[:, :nt], ALU.mult, ALU.mult,
                    [bosq, B_pp, bsgo], [(B_YB, g)])

        def run_interleaved(*pairs):
            live = [[gen, st] for gen, st in pairs]
            t = 0
            while live:
                for item in list(live):
                    if item[1] > t and any(o[1] <= t for o in live):
                        continue
                    try:
                        next(item[0])
                    except StopIteration:
                        live.remove(item)
                t += 1

        def merge(nt):
            for m in range(NCH):
                spa, bspa = load_slab(wcols(w_pa, m * 128))
                pa_, bpa = ps()
                for c in range(NCH):
                    mm(pa_[:, :nt], spa[:, c, :], YA[:, c, :nt], c == 0, c == NCH - 1, [bspa, (B_YA, c)], [bpa])
                sza, bsza = load_slab(wcols(w_in, 12288 + m * 128))
                pza, bpza = proj_fm(sza, bsza, nt)
                za, bza = wtile()
                act(za[:, :nt], pza[:, :nt], AF.Sigmoid, [bpza], [bza])
                tt(za[:, :nt], pa_[:, :nt], za[:, :nt], ALU.mult, [bpa, bza], [bza])
                spb, bspb = load_slab(wcols(w_pb, m * 128))
                pb_, bpb = ps()
                for c in range(NCH):
                    mm(pb_[:, :nt], spb[:, c, :], YB[:, c, :nt], c == 0, c == NCH - 1, [bspb, (B_YB, c)], [bpb])
                szb, bszb = load_slab(wcols(w_in, 14336 + m * 128))
                pzb, bpzb = proj_fm(szb, bszb, nt)
                zb, bzb = wtile()
                act(zb[:, :nt], pzb[:, :nt], AF.Sigmoid, [bpzb], [bzb])
                tt(zb[:, :nt], pb_[:, :nt], zb[:, :nt], ALU.mult, [bpb, bzb], [bzb])
                tt(MIX[:, m, :nt], za[:, :nt], zb[:, :nt], ALU.add, [bza, bzb], [(B_MIX, m)])
            for m in range(NCH):
                swo, bswo = load_slab(wcols(w_out, m * 128))
                pd_, bpd = ps()
                for c in range(NCH):
                    mm(pd_[:, :nt], swo[:, c, :], MIX[:, c, :nt], c == 0, c == NCH - 1, [bswo, (B_MIX, c)], [bpd])
                tt(HT[:, m, :nt], HT[:, m, :nt], pd_[:, :nt], ALU.add, [(B_HT, m), bpd], [(B_HT, m)])

        out_events = []

        def final_out(t0, nt):
            rms_rows(nt, PP_GF, out_is_xt=False)
            for c in range(NCH):
                stt(HT[:, c, :nt], HT[:, c, :nt], pp[:, PP_GF + c:PP_GF + c + 1], rstd[:, :nt],
                    ALU.mult, ALU.mult, [(B_HT, c), B_pp, B_rstd], [(B_HT, c)])
            for ti in range(nt // 128):
                st, bst = xst[ti % 2]
                for c4 in range(4):
                    pt, pbuf = ps()
                    for j in range(4):
                        c = c4 * 4 + j
                        tr(pt[:, j * 128:(j + 1) * 128], HT[:, c, ti * 128:(ti + 1) * 128], ident, [(B_HT, c), B_cst], [pbuf])
                    cp('act', st[:, c4 * 512:(c4 + 1) * 512], pt[:, :], [pbuf], [bst] + XS_ALIAS[ti % 2])
                ev = S.dma('sp', y[t0 + ti * 128:t0 + (ti + 1) * 128, :], st[:, :], reads=[bst], writes=[B_y],
                           chan=('xst', ti % 2))
                out_events.append(ev)

        def vmax(out, in_, reads, writes):
            S.op('dve', lambda e: e.max(out=out, in_=in_), reads, writes)

        def vmaxidx(out, in_max, in_values, reads, writes):
            S.op('dve', lambda e: e.max_index(out=out, in_max=in_max, in_values=in_values), reads, writes)

        def vmrep(out, rep, vals, reads, writes):
            S.op('dve', lambda e: e.match_replace(out=out, in_to_replace=rep, in_values=vals, imm_value=-1e30), reads, writes)

        def vred(out, in_, reads, writes):
            S.op('dve', lambda e: e.tensor_reduce(out=out, in_=in_, axis=AX.X, op=ALU.add), reads, writes)

        def peer(nt):
            ntl = nt // 128
            rms_rows(nt, PP_G2)
            QT = MIX
            for m in range(NCH):
                sq_, bsq = load_slab(wcols(wq, m * 128))
                pq, bpq = proj_fm(sq_, bsq, nt)
                cp('act', QT[:, m, :nt], pq[:, :nt], [bpq], [(B_A3, m)])
            sk, bsk = load_slab(keysT.rearrange("m d k -> d m k"))
            sc = WK[:, 0:2048]
            sc_b = [wk[i][1] for i in range(0, 4)]
            sc2 = WK[:, 4 * WKW:4 * WKW + 2048]
            sc2_b = [wk[i][1] for i in range(4, 8)]
            cand = WK[:, 8 * WKW:8 * WKW + 2048]
            cand_b = [wk[i][1] for i in range(8, 12)]
            sc3 = sc.rearrange("p (m k) -> p m k", k=128)
            sc23 = sc2.rearrange("p (m k) -> p m k", k=128)
            cand4 = cand.rearrange("p (h a b) -> p h a b", a=16, b=16)
            cand3 = cand.rearrange("p (h c) -> p h c", c=256)
            scc3 = sc.rearrange("p (h c) -> p h c", c=256)
            tv4 = tv[:, :].rearrange("p (h q k) -> p h q k", q=2, k=16)
            tif4 = tif[:, :].rearrange("p (h q k) -> p h q k", q=2, k=16)
            bv3 = bv[:, :].rearrange("p (h k) -> p h k", k=16)
            g3 = pk[:, 5, :].rearrange("p (h k) -> p h k", k=16)
            ge3 = sc2.rearrange("p (x j) -> p x j", j=16)
            eq4 = sc2.rearrange("p (h k a) -> p h k a", k=16, a=16)
            iota16b = cst[:, C_IOTA16:C_IOTA16 + 16].unsqueeze(1).unsqueeze(1).to_broadcast([128, 8, 16, 16])
            thr16b = cst[:, C_THR16:C_THR16 + 16].unsqueeze(1).to_broadcast([128, 128, 16])
            for ti in range(ntl):
                tsl = slice(ti * 128, (ti + 1) * 128)
                for b4 in range(4):
                    pt, pbuf = ps()
                    for j in range(4):
                        m = b4 * 4 + j
                        mm(pt[:, j * 128:(j + 1) * 128], QT[:, m, tsl], sk[:, m, :], True, True, [(B_A3, m), bsk], [pbuf])
                    cp('act', sc[:, b4 * 512:(b4 + 1) * 512], pt[:, :], [pbuf], sc_b)
                t0_ = lambda m: tv[:, m * 16:m * 16 + 8]
                t1_ = lambda m: tv[:, m * 16 + 8:m * 16 + 16]
                for m in range(16):
                    vmax(t0_(m), sc3[:, m, :], sc_b, [(B_tv, m)])
                for m in range(16):
                    vmaxidx(tix[:, m * 16:m * 16 + 8], t0_(m), sc3[:, m, :], sc_b + [(B_tv, m)], [(B_tix, m)])
                for m in range(16):
                    vmrep(sc23[:, m, :], t0_(m), sc3[:, m, :], sc_b + [(B_tv, m)], sc2_b)
                for m in range(16):
                    vmax(t1_(m), sc23[:, m, :], sc2_b, [(B_tv, m)])
                for m in range(16):
                    vmaxidx(tix[:, m * 16 + 8:m * 16 + 16], t1_(m), sc23[:, m, :], sc2_b + [(B_tv, m)], [(B_tix, m)])
                cp('dve', tif[:, :], tix[:, :], [B_tix], [B_tif])
                tt(cand4, tv4[:, :, 0, :].unsqueeze(3).to_broadcast([128, 8, 16, 16]),
                   tv4[:, :, 1, :].unsqueeze(2).to_broadcast([128, 8, 16, 16]), ALU.add, [B_tv], cand_b)
                b0_ = lambda h: bv[:, h * 16:h * 16 + 8]
                b1_ = lambda h: bv[:, h * 16 + 8:h * 16 + 16]
                for h in range(8):
                    vmax(b0_(h), cand3[:, h, :], cand_b, [(B_bv, h)])
                for h in range(8):
                    vmaxidx(bpos[:, h * 16:h * 16 + 8], b0_(h), cand3[:, h, :], cand_b + [(B_bv, h)], [(B_bpos, h)])
                for h in range(8):
                    vmrep(scc3[:, h, :], b0_(h), cand3[:, h, :], cand_b + [(B_bv, h)], sc_b)
                for h in range(8):
                    vmax(b1_(h), scc3[:, h, :], sc_b, [(B_bv, h)])
                for h in range(8):
                    vmaxidx(bpos[:, h * 16 + 8:h * 16 + 16], b1_(h), scc3[:, h, :], sc_b + [(B_bv, h)], [(B_bpos, h)])
                tt(g3, bv3, bv3[:, :, 0:1].to_broadcast([128, 8, 16]), ALU.subtract, [B_bv], [(B_pk, 5)])
                act(pk[:, 5, :], pk[:, 5, :], AF.Exp, [(B_pk, 5)], [(B_pk, 5)])
                vred(hs8[:, 0:8], g3, [(B_pk, 5)], [B_hs8])
                S.op('dve', lambda e: e.reciprocal(out=hs8[:, 0:8], in_=hs8[:, 0:8]), [B_hs8], [B_hs8])
                tt(g3, g3, hs8[:, 0:8].unsqueeze(2).to_broadcast([128, 8, 16]), ALU.mult, [(B_pk, 5), B_hs8], [(B_pk, 5)])
                cp('dve', pk[:, 0, :], bpos[:, :], [B_bpos], [(B_pk, 0)])
                tt(ge3, pk[:, 0, :].unsqueeze(2).to_broadcast([128, 128, 16]), thr16b, ALU.is_ge, [(B_pk, 0), B_cst], sc2_b)
                vred(pk[:, 1, :], ge3, sc2_b, [(B_pk, 1)])
                stt(pk[:, 2, :], pk[:, 1, :], -16.0, pk[:, 0, :], ALU.mult, ALU.add, [(B_pk, 1), (B_pk, 0)], [(B_pk, 2)])
                for q in range(2):
                    ab3 = pk[:, 1 + q, :].rearrange("p (h k) -> p h k", k=16)
                    tt(eq4, ab3.unsqueeze(3).to_broadcast([128, 8, 16, 16]), iota16b, ALU.is_equal, [(B_pk, 1 + q), B_cst], sc2_b)
                    tt(eq4, eq4, tif4[:, :, q, :].unsqueeze(2).to_broadcast([128, 8, 16, 16]), ALU.mult, sc2_b + [B_tif], sc2_b)
                    vred(pk[:, 3 + q, :].rearrange("p (h k) -> p h k", k=16), eq4, sc2_b, [(B_pk, 3 + q)])
                pt, pbuf = ps()
                for q in range(3):
                    tr(pt[:, q * 128:(q + 1) * 128], pk[:, 3 + q, :], ident, [(B_pk, 3 + q), B_cst], [pbuf])
                cp('act', ijg[:, ti, :, :], pt[:, 0:384].rearrange("p (q n) -> p q n", n=128), [pbuf], [(B_ijg, ti)])
            GOIb = [AR1[:, k * 4096:(k + 1) * 4096].rearrange("p (n i) -> p n i", i=128) for k in range(2)]
            OJb = [AR2[:, k * 4096:(k + 1) * 4096].rearrange("p (n i) -> p n i", i=128) for k in range(2)]
            Wt_v = AR3[:, :].rearrange("p (g n c) -> p n g c", n=128, c=4)
            iota_b = cst[:, C_IOTA:C_IOTA + 128].unsqueeze(1).to_broadcast([128, 32, 128])
            S.op('dve', lambda e: e.memset(hs8[:, 8:9], 0.0), [], [B_YA, B_YB, B_hs8])
            sub = 0
            for ti in range(ntl):
                for sb_ in range(4):
                    k = sub % 2
                    sub += 1
                    ns = slice(sb_ * 32, (sb_ + 1) * 32)
                    iTs = ijg[:, ti, 0, ns].unsqueeze(2).to_broadcast([128, 32, 128])
                    jTs = ijg[:, ti, 1, ns].unsqueeze(2).to_broadcast([128, 32, 128])
                    gTs = ijg[:, ti, 2, ns].unsqueeze(2).to_broadcast([128, 32, 128])
                    bgoi = (B_YA, ('goi', k))
                    boj = (B_YB, ('oj', k))
                    tt(GOIb[k], iota_b, iTs, ALU.is_equal, [B_cst, (B_ijg, ti)], [bgoi])
                    tt(GOIb[k], GOIb[k], gTs, ALU.mult, [bgoi, (B_ijg, ti)], [bgoi])
                    tt(OJb[k], iota_b, jTs, ALU.is_equal, [B_cst, (B_ijg, ti)], [boj])
                    for n in range(32):
                        if n % 4 == 0:
                            pt, pbuf = ps()
                        mm(pt[:, (n % 4) * 128:(n % 4 + 1) * 128], GOIb[k][:, n, :], OJb[k][:, n, :], True, True, [bgoi, boj], [pbuf])
                        if n % 4 == 3:
                            ntok = sb_ * 32 + n
                            cp('act', Wt_v[:, ntok - 3:ntok + 1, :, :],
                               pt[:, :].rearrange("p (n g c) -> p n g c", g=32, c=4), [pbuf], [B_A3])
                S.dma('sp', wd_scr[ti, :, :], AR3[:, :], reads=[B_A3], writes=[(B_wd, ti)], chan='wdw')
            S.op('dve', lambda e: e.memset(hs8[:, 8:9], 0.0), [], [B_YA, B_YB, B_hs8])
            vring = [AR1[:, k * 2048:(k + 1) * 2048].rearrange("p (m d) -> p m d", d=128) for k in range(4)] + \
                    [AR2[:, k * 2048:(k + 1) * 2048].rearrange("p (m d) -> p m d", d=128) for k in range(4)]
            vring_b = [(B_YA, ('vs', k)) for k in range(4)] + [(B_YB, ('vs', k)) for k in range(4)]

            def stageA(grp):
                key = 'wg%d' % (grp % 2)
                wgt = AR3[:, (grp % 2) * 2048:(grp % 2) * 2048 + 2048].rearrange("p (t x) -> p t x", x=512)
                S.dma('sp', wgt[:, 0:ntl, :],
                      wd_scr.rearrange("t p (g x) -> p g t x", x=512)[:, grp, 0:ntl, :],
                      reads=[B_wd], writes=[(B_A3, key)], chan=('wg', grp % 2))
                wgv = AR3[:, (grp % 2) * 2048:(grp % 2) * 2048 + 2048].rearrange("p (tn c) -> p tn c", c=4)
                was, vs = [], []
                us = [load_slab(wcols(uT, (grp * 4 + j) * 128)) for j in range(4)]
                for j in range(4):
                    su, bsu = us[j]
                    pa_, bpa = proj_fm(su, bsu, nt)
                    k = (grp % 2) * 4 + j
                    ga = AR3[:, 4096 + k * 512:4096 + (k + 1) * 512]
                    bga = (B_A3, ('wa', k))
                    act(ga[:, :nt], pa_[:, :nt], AF.Gelu, [bpa], [bga])
                    tt(ga[:, :nt], ga[:, :nt], wgv[:, :nt, j], ALU.mult, [bga, (B_A3, key)], [bga])
                    was.append((ga, bga))
                for j in range(4):
                    c = grp * 4 + j
                    k = (grp % 2) * 4 + j
                    S.dma('pool', vring[k][:, :, :], vP[c * 128:(c + 1) * 128, :].rearrange("p (m d) -> p m d", d=128),
                          reads=[], writes=[vring_b[k]], chan=('vring', k))
                    vs.append((vring[k], vring_b[k]))
                return was, vs

            def stageB(was, vs):
                for m in range(NCH):
                    po_, bpo_ = ps()
                    for j in range(4):
                        mm(po_[:, :nt], vs[j][0][:, m, :], was[j][0][:, :nt], j == 0, j == 3, [vs[j][1], was[j][1]], [bpo_])
                    tt(HT[:, m, :nt], HT[:, m, :nt], po_[:, :nt], ALU.add, [(B_HT, m), bpo_], [(B_HT, m)])

            cur = stageA(0)
            for grp in range(32):
                nxt = stageA(grp + 1) if grp + 1 < 32 else None
                stageB(*cur)
                cur = nxt

        t0 = 0
        for nt in pre_blocks:
            S.dma('sp', mpre[:, :nt], maskpre[:, t0:t0 + nt], writes=[B_mpre], chan='mpre')
            load_block(xpre, t0, nt)
            rms_rows(nt, PP_G1)
            slab_mode[0] = 'pre'
            if PRE_PAIR:
                for g in range(0, 16, 2):
                    run_interleaved((rglru_head(g, nt, False, t0, R0pre), 0), (hgrn2_head(g, nt, False, R0pre), HG_OFFSET),
                                    (rglru_head(g + 1, nt, False, t0, R1), PAIR_OFFSET),
                                    (hgrn2_head(g + 1, nt, False, R1), PAIR_OFFSET + HG_OFFSET))
            else:
                for g in range(16):
                    run_interleaved((rglru_head(g, nt, False, t0), 0), (hgrn2_head(g, nt, False), HG_OFFSET))
            slab_mode[0] = 'base'
            t0 += nt
        if PRE_PAIR:
            S.op('dve', lambda e: e.memset(hs8[:, 10:11], 0.0), R1_ALLBUFS, [B_YA, B_YB, B_hs8])
        t0 = 0
        for nt in main_blocks:
            load_block(xmain, t0, nt)
            rms_rows(nt, PP_G1)
            S.op('dve', lambda e: e.memset(hs8[:, 9:10], 0.0), [], [B_A3, B_hs8])
            slab_mode[0] = 'heads'
            for g in range(16):
                run_interleaved((rglru_head(g, nt, True, t0), 0), (hgrn2_head(g, nt, True), HG_OFFSET))
            slab_mode[0] = 'base'
            merge(nt)
            if do_peer:
                peer(nt)
            final_out(t0, nt)
            t0 += nt
        S.final_wait('sp', out_events)
        if dbg:
            print(act_log)
        S.emit()
    return nc


def _pp_layout(v):
    return np.ascontiguousarray(np.asarray(v, np.float32).reshape(16, 128).T)


def make_shared(inp):
    pp = np.zeros((128, NPP), np.float32)
    pp[:, PP_G1:PP_G1 + 16] = _pp_layout(inp['ln1_g'][0])
    pp[:, PP_G2:PP_G2 + 16] = _pp_layout(inp['ln2_g'][0])
    pp[:, PP_GF:PP_GF + 16] = _pp_layout(inp['final_g'])
    cw = np.asarray(inp['conv_w'][0], np.float32)
    pp[:, PP_CW:PP_CW + 64] = np.ascontiguousarray(cw.reshape(4, 16, 128).transpose(2, 1, 0)).reshape(128, 64)
    pp[:, PP_CB:PP_CB + 16] = _pp_layout(inp['conv_b'][0])
    pp[:, PP_BA:PP_BA + 16] = _pp_layout(inp['rg_ba'][0])
    pp[:, PP_BX:PP_BX + 16] = _pp_layout(inp['rg_bx'][0])
    pp[:, PP_LAM:PP_LAM + 16] = _pp_layout(inp['rg_lambda'][0])
    pp[:, PP_L0:PP_L0 + 16] = _pp_layout(inp['hg_lb_logits'][0])
    pp[:, PP_L1:PP_L1 + 16] = _pp_layout(inp['hg_lb_logits'][1])
    pp[:, PP_NG:PP_NG + 16] = _pp_layout(inp['hg_norm_g'][0])
    cs = np.zeros((128, NCONST), np.float32)
    cs[:, C_ID:C_ID + 128] = np.eye(128, dtype=np.float32)
    s = np.arange(64)
    cs[0:64, C_MASKT:C_MASKT + 64] = (s[:, None] <= s[None, :]).astype(np.float32)
    rm = np.ones(512, np.float32)
    rm[::64] = 0.0
    cs[:, C_RESET:C_RESET + 512] = rm[None, :]
    cs[:, C_IOTA:C_IOTA + 128] = np.arange(128, dtype=np.float32)[None, :]
    cs[:, C_ONES:C_ONES + 128] = 1.0
    cs[:, C_EPS] = EPS
    cs[:, C_ONE] = 1.0
    cs[:, C_IOTA16:C_IOTA16 + 16] = np.arange(16, dtype=np.float32)[None, :]
    thr = (np.arange(16, dtype=np.float32) + 1.0) * 16.0
    thr[15] = 1e9
    cs[:, C_THR16:C_THR16 + 16] = thr[None, :]
    keys = np.asarray(inp['peer_keys'][0], np.float32)
    keysT = np.ascontiguousarray(keys.reshape(16, 128, 128).transpose(0, 2, 1))
    u = np.asarray(inp['peer_u'][0], np.float32)
    v = np.asarray(inp['peer_v'][0], np.float32)
    uT = np.ascontiguousarray(u.reshape(128, 128, 16, 128).transpose(1, 3, 2, 0)).reshape(128, 128, D)
    vP = np.ascontiguousarray(v.reshape(128, 128, D).transpose(1, 0, 2).reshape(NEXP, D))
    def slabify(w):
        w = np.asarray(w, np.float32)
        ns = w.shape[1] // 128
        return np.ascontiguousarray(w.reshape(16, 128, ns, 128).transpose(2, 1, 0, 3)).reshape(ns, 128, D)

    return {
        'w_in': slabify(inp['w_in'][0]),
        'w_pa': slabify(inp['w_pa'][0]),
        'w_pb': slabify(inp['w_pb'][0]),
        'w_out': slabify(inp['w_out'][0]),
        'wq': slabify(inp['peer_wq'][0]),
        'rg_wa': np.ascontiguousarray(inp['rg_wa'][0], np.float32),
        'rg_wx': np.ascontiguousarray(inp['rg_wx'][0], np.float32),
        'pp': pp, 'consts': cs, 'keysT': keysT, 'uT': uT, 'vP': vP,
    }


HG_OFFSET = 4
WARM_K = 0
PRE_PAIR = 1
PAIR_OFFSET = 2
RG_GELU_DELAY = 3
PRE_BLOCKS = [512, 512, 512, 512, 128]
MAIN_BLOCKS = [512, 512, 512, 512]


def kernel(**inputs):
    x = np.asarray(inputs['x'], np.float32)
    meta = np.asarray(inputs['meta'], np.float32)
    B, T, _ = x.shape
    half = T // 2
    shared = make_shared(inputs)
    npre = sum(PRE_BLOCKS)
    in_maps = []
    for b in range(B):
        for s in range(2):
            xpre = np.zeros((npre, D), np.float32)
            mask = np.zeros((128, npre), np.float32)
            if s == 0:
                xpre[npre - 16:] = meta
                mask[:, npre - 16:] = 1.0
            else:
                xpre[npre - half - 16:npre - half] = meta
                xpre[npre - half:] = x[b, :half]
                mask[:, npre - half - 16:] = 1.0
            m = dict(shared)
            m['xpre'] = xpre
            m['maskpre'] = mask
            m['xmain'] = np.ascontiguousarray(x[b, s * half:(s + 1) * half])
            in_maps.append(m)
    nc = build_program(PRE_BLOCKS, MAIN_BLOCKS, do_peer=True)
    res = run_bass_kernel_spmd(nc, in_maps, core_ids=list(range(8)))
    out = np.zeros((B, T, D), np.float32)
    i = 0
    for b in range(B):
        for s in range(2):
            out[b, s * half:(s + 1) * half] = res.results[i]['y']
            i += 1
    return out
```

```python
import numpy as np
from contextlib import ExitStack
import concourse.bass as bass
import concourse.mybir as mybir
from concourse.bass_utils import run_bass_kernel_spmd

F32 = mybir.dt.float32
BF16 = mybir.dt.bfloat16
U32 = mybir.dt.uint32
I32 = mybir.dt.int32
AF = mybir.ActivationFunctionType
ALU = mybir.AluOpType
AX = mybir.AxisListType

D = 2048
NCH = 16
TB = 512
CH = 64
EPS = 1e-6
NKEY = 128
NEXP = 16384

PP_G1, PP_G2, PP_GF = 0, 16, 32
PP_CW = 48
PP_CB = 112
PP_BA = 128
PP_BX = 144
PP_LAM = 160
PP_L0 = 176
PP_L1 = 192
PP_NG = 208
NPP = 224
C_ID = 0
C_MASKT = 128
C_RESET = 192
C_IOTA = 704
C_ONES = 832
C_EPS = 960
C_ONE = 961
C_IOTA16 = 962
C_THR16 = 978
NCONST = 994


class Buf:
    def __init__(self, name):
        self.name = name
        self.w = {}
        self.r = {}

    def _keys(self, k):
        if k == '*':
            return list(set(self.w) | set(self.r) | {'*'})
        return [k, '*']

    def rdeps(self, k):
        return [self.w[kk] for kk in self._keys(k) if kk in self.w]

    def wdeps(self, k):
        waw = [self.w[kk] for kk in self._keys(k) if kk in self.w]
        war = []
        for kk in self._keys(k):
            for sk, v in self.r.get(kk, {}).items():
                war.append((sk, v))
        return waw, war

    def did_read(self, k, ev):
        d = self.r.setdefault(k, {})
        d[ev[0]] = max(d.get(ev[0], 0), ev[1])

    def did_write(self, k, ev):
        if k == '*':
            self.w = {'*': ev}
            self.r = {}
        else:
            self.w[k] = ev
            self.r[k] = {}


def _norm(lst):
    out = []
    for x in lst:
        if isinstance(x, Buf):
            out.append((x, '*'))
        elif isinstance(x, list):
            out.extend(_norm(x))
        else:
            out.append(x)
    return out


class Sched:
    EPOCH = 30000
    DMA_EPOCH = 1800

    def __init__(self, nc, stack):
        self.nc = nc
        self.stack = stack
        self.eng = {'pe': nc.tensor, 'act': nc.scalar, 'dve': nc.vector, 'pool': nc.gpsimd, 'sp': nc.sync}
        self.prog = {k: [] for k in self.eng}
        self.sems = {}
        self.cnt = {}
        self.epoch = {}
        self.waited = {k: {} for k in self.eng}
        self.nsem = 0

    def _sem(self, key):
        if key not in self.sems:
            self.nsem += 1
            self.sems[key] = self.stack.enter_context(self.nc.semaphore("s%d" % self.nsem))
            self.cnt[key] = 0
        return self.sems[key]

    def _next_event(self, base, inc, limit):
        ep = self.epoch.get(base, 0)
        key = (base, ep)
        self._sem(key)
        if self.cnt[key] + inc > limit:
            ep += 1
            self.epoch[base] = ep
            key = (base, ep)
            self._sem(key)
        self.cnt[key] += inc
        return (key, self.cnt[key])

    def _waits(self, eng, reads, writes):
        deps = []
        for b, k in reads:
            for ev in b.rdeps(k):
                deps.append((ev, 'raw'))
        for b, k in writes:
            waw, war = b.wdeps(k)
            for ev in waw:
                deps.append((ev, 'waw'))
            for ev in war:
                deps.append((ev, 'war'))
        waits = []
        wd = self.waited[eng]
        for (sk, v), kind in deps:
            base = sk[0]
            if base == ('eng', eng):
                if eng == 'pe' or kind != 'raw':
                    continue
            if wd.get(sk, 0) >= v:
                continue
            wd[sk] = v
            waits.append((sk, v))
        return waits

    def op(self, eng, fn, reads=(), writes=()):
        reads = _norm(reads)
        writes = _norm(writes)
        waits = self._waits(eng, reads, writes)
        ev = self._next_event(('eng', eng), 1, self.EPOCH)
        self.prog[eng].append((waits, fn, ev[0], 1))
        for b, k in reads:
            b.did_read(k, ev)
        for b, k in writes:
            b.did_write(k, ev)
        return ev

    def dma(self, q, out, in_, reads=(), writes=(), chan=None):
        reads = _norm(reads)
        writes = _norm(writes)
        waits = self._waits(q, reads, writes)
        ev = self._next_event(('dma', chan), 16, self.DMA_EPOCH * 16)
        self.prog[q].append((waits, lambda e: e.dma_start(out=out, in_=in_), ev[0], 16))
        for b, k in reads:
            b.did_read(k, ev)
        for b, k in writes:
            b.did_write(k, ev)
        return ev

    def final_wait(self, eng, events):
        waits = []
        for sk, v in events:
            waits.append((sk, v))
        self.prog[eng].append((waits, None, None, 0))

    def emit(self):
        nc = self.nc
        with nc.Block() as block:
            decs = {'pe': block.tensor, 'act': block.scalar, 'dve': block.vector,
                    'pool': block.gpsimd, 'sp': block.sync}
            for name in ['sp', 'pool', 'pe', 'act', 'dve']:
                lst = self.prog[name]
                if not lst:
                    continue

                def body(e, lst=lst):
                    for waits, fn, semkey, inc in lst:
                        for sk, v in waits:
                            e.wait_ge(self.sems[sk], v)
                        if fn is not None:
                            r = fn(e)
                            r.then_inc(self.sems[semkey], inc)
                decs[name](body)


def build_program(pre_blocks, main_blocks, do_peer=True, dbg=False):
    nc = bass.Bass("TRN2", target_bir_lowering=False)
    npre = sum(pre_blocks)
    nmain = sum(main_blocks)

    def din(name, shape, dt=F32):
        return nc.dram_tensor(name, list(shape), dt, kind="ExternalInput").ap()

    xpre = din("xpre", [max(npre, 128), D])
    xmain = din("xmain", [nmain, D])
    maskpre = din("maskpre", [128, max(npre, 128)])
    w_in = din("w_in", [128, 128, D])
    w_pa = din("w_pa", [16, 128, D])
    w_pb = din("w_pb", [16, 128, D])
    w_out = din("w_out", [16, 128, D])
    wq = din("wq", [16, 128, D])
    rg_wa = din("rg_wa", [16, 128, 128])
    rg_wx = din("rg_wx", [16, 128, 128])
    pp_d = din("pp", [128, NPP])
    consts_d = din("consts", [128, NCONST])
    keysT = din("keysT", [16, 128, 128])
    uT = din("uT", [128, 128, D])
    vP = din("vP", [NEXP, D])
    y = nc.dram_tensor("y", [nmain, D], F32, kind="ExternalOutput").ap()
    wd_scr = nc.dram_tensor("wd_scr", [4, 128, 128 * 128], BF16, kind="Internal").ap()

    stack = ExitStack()
    with stack:
        S = Sched(nc, stack)
        bufs = {}

        def sb(name, shape, dt=F32):
            t = stack.enter_context(nc.sbuf_tensor("s_" + name, list(shape), dt))
            b = Buf(name)
            bufs[name] = b
            return t, b

        def psb(name, shape, dt=F32):
            t = stack.enter_context(nc.psum_tensor("p_" + name, list(shape), dt))
            b = Buf(name)
            return t, b

        B_y = Buf("y")
        B_wd = Buf("wd")

        HT, B_HT = sb("HT", [128, NCH, TB], F32)
        XT, B_XT = sb("XT", [128, NCH, TB], BF16)
        AR1, B_YA = sb("AR1", [128, NCH * TB], BF16)
        AR2, B_YB = sb("AR2", [128, NCH * TB], BF16)
        AR3, B_A3 = sb("AR3", [128, 2 * NCH * TB], BF16)
        YA = AR1[:, :].rearrange("p (c t) -> p c t", t=TB)
        YB = AR2[:, :].rearrange("p (c t) -> p c t", t=TB)
        MIX = AR3[:, 0:NCH * TB].rearrange("p (c t) -> p c t", t=TB)
        B_MIX = B_A3
        _xs = AR3[:, NCH * TB:2 * NCH * TB].bitcast(F32)
        xst = [(_xs[:, 0:D], (B_A3, 'x0')), (_xs[:, D:2 * D], (B_A3, 'x1'))]
        XS_ALIAS = {0: [(B_A3, ('hg', i)) for i in (0, 1, 2, 3)], 1: [(B_A3, ('hg', i)) for i in (3, 4, 5, 6)]}
        NSLAB = 6
        slabs = [sb("slab%d" % i, [128, NCH, 128], BF16) for i in range(NSLAB)]
        pp, B_pp = sb("pp", [128, NPP], F32)
        cst, B_cst = sb("cst", [128, NCONST], F32)
        identb, B_identb = sb("identb", [128, 128], BF16)
        maskTb, B_maskTb = sb("maskTb", [64, 64], F32)
        drv, B_drv = sb("drv", [128, 96], F32)
        wab, B_wab = sb("wab", [128, 2, 2, 128], BF16)
        xa_tail, B_xat = sb("xa_tail", [128, 16, 4], F32)
        hcar, B_hcar = sb("hcar", [128, 16], F32)
        Sst, B_S = sb("Sst", [128, 16, 128], F32)
        Sall, B_Sall = sb("Sall", [128, 8, 128], F32)
        Sbf8, B_Sbf8 = sb("Sbf8", [128, 8, 128], BF16)
        mpre, B_mpre = sb("mpre", [128, TB], F32)
        rstd, B_rstd = sb("rstd", [128, TB], F32)
        NW = 12
        WKW = TB + 4
        WK, _ = sb("WK", [128, NW * WKW], F32)
        wk = [(WK[:, i * WKW:(i + 1) * WKW], Buf("wk%d" % i)) for i in range(NW)]
        NWB = 6
        wkb = [sb("wkb%d" % i, [128, TB], BF16) for i in range(NWB)]
        vbf, B_vbf = sb("vbf", [64, 8, 128], BF16)
        scm8, B_scm8 = sb("scm8", [64, 8, 64], BF16)
        ket8, B_ket8 = sb("ket8", [64, 8, 128], BF16)
        sml, B_sml = sb("sml", [128, 64], F32)
        tv, B_tv = sb("tv", [128, 256], F32)
        tix, B_tix = sb("tix", [128, 256], U32)
        tif, B_tif = sb("tif", [128, 256], F32)
        bv, B_bv = sb("bv", [128, 128], F32)
        bpos, B_bpos = sb("bpos", [128, 128], U32)
        pk, B_pk = sb("pk", [128, 6, 128], F32)
        hs8, B_hs8 = sb("hs8", [128, 16], F32)
        ijg, B_ijg = sb("ijg", [128, 4, 3, 128], BF16)

        pbanks = [psb("pb%d" % i, [128, 512], F32) for i in range(7)]
        pbb, B_pbb = psb("pbb", [128, 1024], BF16)
        rot = [0]

        def ps():
            i = 1 + (rot[0] % 6)
            rot[0] += 1
            return pbanks[i]

        wrot = [0]

        def wtile():
            i = wrot[0] % NW
            wrot[0] += 1
            return wk[i]

        wbrot = [0]

        def wbtile():
            i = wbrot[0] % NWB
            wbrot[0] += 1
            return wkb[i]

        srot = {'base': 0, 'heads': 0, 'pre': 0}
        slab_mode = ['base']
        ex_mix = [(AR3[:, j * 2048:(j + 1) * 2048].rearrange("p (c n) -> p c n", n=128),
                   [(B_A3, 4 * j + i) for i in range(4)]) for j in range(4)]
        ex_ya = [(AR1[:, j * 2048:(j + 1) * 2048].rearrange("p (c n) -> p c n", n=128),
                  [(B_YA, 4 * j + i) for i in range(4)]) for j in range(4)]
        ex_yb = [(AR2[:, j * 2048:(j + 1) * 2048].rearrange("p (c n) -> p c n", n=128),
                  [(B_YB, 4 * j + i) for i in range(4)]) for j in range(4)]
        rings = {'base': [(t, [b]) for (t, b) in slabs],
                 'heads': [(t, [b]) for (t, b) in slabs] + ex_mix,
                 'pre': [(t, [b]) for (t, b) in slabs] + ex_mix + ([] if PRE_PAIR else ex_ya + ex_yb)}

        def load_slab(src_ap):
            mode = slab_mode[0]
            ring = rings[mode]
            i = srot[mode] % len(ring)
            srot[mode] += 1
            t, b = ring[i]
            S.dma('pool', t[:, :, :], src_ap, reads=[], writes=b, chan=('slab', i))
            return t, b

        def wcols(w, c0):
            return w[c0 // 128, :, :].rearrange("p (c n) -> p c n", n=128)

        act_log = []

        def act(out, in_, func, reads, writes, bias=None, scale=None):
            act_log.append(str(func))
            kw = {}
            if bias is not None:
                kw['bias'] = bias
            if scale is not None:
                kw['scale'] = scale
            S.op('act', lambda e: e.activation(out=out, in_=in_, func=func, **kw), reads, writes)

        def tt(out, a, b, op, reads, writes, eng='dve'):
            S.op(eng, lambda e: e.tensor_tensor(out=out, in0=a, in1=b, op=op), reads, writes)

        def ts(out, a, s1, s2, op0, op1, reads, writes, eng='dve'):
            if op1 is None:
                S.op(eng, lambda e: e.tensor_scalar(out=out, in0=a, scalar1=s1, scalar2=None, op0=op0), reads, writes)
            else:
                S.op(eng, lambda e: e.tensor_scalar(out=out, in0=a, scalar1=s1, scalar2=s2, op0=op0, op1=op1), reads, writes)

        def stt(out, a, s, b, op0, op1, reads, writes):
            S.op('dve', lambda e: e.scalar_tensor_tensor(out=out, in0=a, scalar=s, in1=b, op0=op0, op1=op1), reads, writes)

        def cp(eng, out, in_, reads, writes):
            if eng == 'act':
                S.op('act', lambda e: e.copy(out=out, in_=in_), reads, writes)
            else:
                S.op(eng, lambda e: e.tensor_copy(out=out, in_=in_), reads, writes)

        def mm(out, lhsT, rhs, start, stop, reads, writes):
            S.op('pe', lambda e: e.matmul(out, lhsT, rhs, start=start, stop=stop), reads, writes)

        def tr(out, in_, ident, reads, writes):
            S.op('pe', lambda e: e.transpose(out, in_, ident), reads, writes)

        S.dma('sp', pp[:, :], pp_d[:, :], writes=[B_pp], chan='pp')
        S.dma('sp', cst[:, :], consts_d[:, :], writes=[B_cst], chan='cst')
        cp('dve', identb[:, :], cst[:, C_ID:C_ID + 128], [B_cst], [B_identb])
        cp('dve', maskTb[:, :], cst[0:64, C_MASKT:C_MASKT + 64], [B_cst], [B_maskTb])
        onesb, B_onesb = sb("onesb", [128, 128], BF16)
        cp('dve', onesb[:, :], cst[:, C_ONES:C_ONES + 128], [B_cst], [B_onesb])
        ident = cst[:, C_ID:C_ID + 128]
        ones_f = cst[:, C_ONES:C_ONES + 128]
        eps_c = cst[:, C_EPS:C_EPS + 1]
        one_c = cst[:, C_ONE:C_ONE + 1]
        act(drv[:, 48:64], pp[:, PP_LAM:PP_LAM + 16], AF.Exp, [B_pp], [(B_drv, 't')], scale=-1.0)
        act(drv[:, 48:64], drv[:, 48:64], AF.Ln, [(B_drv, 't'), B_cst], [(B_drv, 't')], bias=one_c, scale=1.0)
        ts(drv[:, 0:16], drv[:, 48:64], -8.0, None, ALU.mult, None, [(B_drv, 't')], [(B_drv, 'c1')])
        tt(drv[:, 48:64], pp[:, PP_L0:PP_L0 + 16], pp[:, PP_L1:PP_L1 + 16], ALU.subtract, [B_pp, (B_drv, 'c1')], [(B_drv, 't')])
        act(drv[:, 16:32], drv[:, 48:64], AF.Sigmoid, [(B_drv, 't')], [(B_drv, 'lb')])
        ts(drv[:, 32:48], drv[:, 16:32], -1.0, 1.0, ALU.mult, ALU.add, [(B_drv, 'lb')], [(B_drv, 'oml')])
        ts(drv[:, 48:64], drv[:, 32:48], -1.0, None, ALU.mult, None, [(B_drv, 'oml'), (B_drv, 't')], [(B_drv, 't')])
        ts(drv[:, 64:80], drv[:, 0:16], 2.0, None, ALU.mult, None, [(B_drv, 'c1')], [(B_drv, 'c2')])
        S.op('dve', lambda e: e.memset(xa_tail[:, :, :], 0.0), [], [B_xat])
        S.op('dve', lambda e: e.memset(hcar[:, :], 0.0), [], [B_hcar])
        S.op('dve', lambda e: e.memset(Sst[:, :, :], 0.0), [], [B_S])
        B_drvall = B_drv

        def rms_rows(nt, gcol, out_is_xt=True):
            pt, pbuf = ps()
            for c in range(NCH):
                w, bw = wtile()
                wb_ = w.bitcast(BF16)
                act(wb_[:, :nt], HT[:, c, :nt], AF.Square, [(B_HT, c)], [bw])
                mm(pt[:, :nt], onesb[:, :], wb_[:, :nt], c == 0, c == NCH - 1, [bw, B_onesb], [pbuf])
            act(rstd[:, :nt], pt[:, :nt], AF.Ln, [pbuf, B_cst], [B_rstd], bias=eps_c, scale=1.0 / D)
            act(rstd[:, :nt], rstd[:, :nt], AF.Exp, [B_rstd], [B_rstd], scale=-0.5)
            if out_is_xt:
                for c in range(NCH):
                    stt(XT[:, c, :nt], HT[:, c, :nt], pp[:, gcol + c:gcol + c + 1], rstd[:, :nt],
                        ALU.mult, ALU.mult, [(B_HT, c), B_pp, B_rstd], [(B_XT, c)])

        def load_block(xsrc, t0, nt):
            for ti in range(nt // 128):
                st, bst = xst[ti % 2]
                S.dma('sp', st[:, :], xsrc[t0 + ti * 128:t0 + (ti + 1) * 128, :], writes=[bst] + XS_ALIAS[ti % 2], chan=('xst', ti % 2))
                for c4 in range(4):
                    pt, pbuf = ps()
                    for j in range(4):
                        c = c4 * 4 + j
                        tr(pt[:, j * 128:(j + 1) * 128], st[:, c * 128:(c + 1) * 128], ident, [bst, B_cst], [pbuf])
                    cp('act', HT[:, c4 * 4:(c4 + 1) * 4, ti * 128:(ti + 1) * 128],
                       pt[:, :].rearrange("p (j t) -> p j t", j=4), [pbuf],
                       [(B_HT, c4 * 4 + j) for j in range(4)])

        def proj_fm(slab, bslab, nt):
            pt, pbuf = ps()
            for c in range(NCH):
                mm(pt[:, :nt], slab[:, c, :], XT[:, c, :nt], c == 0, c == NCH - 1, [bslab, (B_XT, c)], [pbuf])
            return pt, pbuf

        rg_ps_rot = [0]

        def ps_rg():
            i = 1 + (rg_ps_rot[0] % 3)
            rg_ps_rot[0] += 1
            return pbanks[i]

        hg_ps_rot = [0]

        def ps_hg():
            i = 4 + (hg_ps_rot[0] % 3)
            hg_ps_rot[0] += 1
            return pbanks[i]

        _hx = AR3[:, NCH * TB:2 * NCH * TB].bitcast(F32)
        htiles = [(_hx[:, i * WKW:(i + 1) * WKW], (B_A3, ('hg', i))) for i in range(7)]
        hrot = [0]

        def htile():
            i = hrot[0] % 7
            hrot[0] += 1
            return htiles[i]

        def _mk_rot(banks):
            st = [0]

            def f():
                i = banks[st[0] % len(banks)]
                st[0] += 1
                return pbanks[i]
            return f

        R0 = {'wt': wtile, 'xcb': wkb[0], 'kd': wkb[1], 'ke': wkb[2], 'vT': wkb[5],
              'vbf': (vbf, B_vbf), 'ket8': (ket8, B_ket8), 'Sall': (Sall, B_Sall), 'sml': (sml, B_sml),
              'ps_rg': ps_rg, 'ps_hg': ps_hg, 'one_bank': False}
        _a1 = AR1[:, :].bitcast(F32)
        r1_tiles = [(_a1[:, i * WKW:(i + 1) * WKW], Buf("r1t%d" % i)) for i in range(7)]
        r1_rot = [0]

        def r1_wt():
            i = r1_rot[0] % 7
            r1_rot[0] += 1
            return r1_tiles[i]

        r1_b16 = [(AR2[:, i * 512:(i + 1) * 512], Buf("r1b%d" % i)) for i in range(4)]
        r1_vbf = (AR2[0:64, 2048:3072].rearrange("p (c v) -> p c v", v=128), Buf("r1vbf"))
        r1_ket8 = (AR2[0:64, 3072:4096].rearrange("p (c v) -> p c v", v=128), Buf("r1ket8"))
        r1_Sall = (AR2[:, 4096:6144].bitcast(F32).rearrange("p (c v) -> p c v", v=128), Buf("r1Sall"))
        r1_sml = (AR2[:, 6144:6272].bitcast(F32), Buf("r1sml"))
        R1 = {'wt': r1_wt, 'xcb': r1_b16[0], 'kd': r1_b16[1], 'ke': r1_b16[2], 'vT': r1_b16[3],
              'vbf': r1_vbf, 'ket8': r1_ket8, 'Sall': r1_Sall, 'sml': r1_sml,
              'ps_rg': _mk_rot([3, 4]), 'ps_hg': _mk_rot([0]), 'one_bank': True}
        R0pre = dict(R0)
        R0pre.update({'ps_rg': _mk_rot([1, 2]), 'ps_hg': _mk_rot([5, 6]), 'one_bank': True})
        R1_ALLBUFS = [b for _, b in r1_tiles] + [b for _, b in r1_b16] + [r1_vbf[1], r1_ket8[1], r1_Sall[1], r1_sml[1]]

        B_dum = Buf("dummy_bank")

        def proj_fm2(slab, bslab, nt, psf):
            pt, pbuf = psf()
            for c in range(NCH):
                mm(pt[:, :nt], slab[:, c, :], XT[:, c, :nt], c == 0, c == NCH - 1, [bslab, (B_XT, c)], [pbuf])
            if WARM_K and slab_mode[0] == 'pre' and not PRE_PAIR:
                for _ in range(WARM_K):
                    mm(pbanks[0][0][:, :nt], identb[:, :], XT[:, 0, :nt], True, True, [B_identb, (B_XT, 0)], [B_dum])
            return pt, pbuf

        def rglru_head(g, nt, main, t0, R=None):
            R = R or R0
            sx, bsx = load_slab(wcols(w_in, g * 128))
            px, bpx = proj_fm2(sx, bsx, nt, R['ps_rg'])
            xe, bxe = R['wt']()
            cp('dve', xe[:, 0:3], xa_tail[:, g, 0:3], [B_xat], [bxe])
            cp('act', xe[:, 3:3 + nt], px[:, :nt], [bpx], [bxe])
            cp('dve', xa_tail[:, g, 0:3], xe[:, nt:nt + 3], [bxe], [B_xat])
            yield
            xc, bxc = R['wt']()
            cw = lambda k: pp[:, PP_CW + g * 4 + k:PP_CW + g * 4 + k + 1]
            ts(xc[:, :nt], xe[:, 3:3 + nt], cw(3), pp[:, PP_CB + g:PP_CB + g + 1], ALU.mult, ALU.add, [bxe, B_pp], [bxc])
            yield
            xcb, bxcb = R['xcb']
            for k in range(3):
                if k < 2:
                    stt(xc[:, :nt], xe[:, k:k + nt], cw(k), xc[:, :nt], ALU.mult, ALU.add, [bxe, B_pp, bxc], [bxc])
                else:
                    stt(xcb[:, :nt], xe[:, k:k + nt], cw(k), xc[:, :nt], ALU.mult, ALU.add, [bxe, B_pp, bxc], [bxcb])
                yield
            wsl = g % 2
            S.dma('pool', wab[:, wsl, 0, :], rg_wa[g, :, :], writes=[(B_wab, wsl)], chan=('wab', wsl))
            S.dma('pool', wab[:, wsl, 1, :], rg_wx[g, :, :], writes=[(B_wab, wsl)], chan=('wab', wsl))
            yield
            pr, bpr = R['ps_rg']()
            mm(pr[:, :nt], wab[:, wsl, 0, :], xcb[:, :nt], True, True, [(B_wab, wsl), bxcb], [bpr])
            pi, bpi = R['ps_rg']()
            mm(pi[:, :nt], wab[:, wsl, 1, :], xcb[:, :nt], True, True, [(B_wab, wsl), bxcb], [bpi])
            yield
            r, br = R['wt']()
            act(r[:, :nt], pr[:, :nt], AF.Sigmoid, [bpr, B_pp], [br], bias=pp[:, PP_BA + g:PP_BA + g + 1], scale=1.0)
            ig, big = R['wt']()
            act(ig[:, :nt], pi[:, :nt], AF.Sigmoid, [bpi, B_pp], [big], bias=pp[:, PP_BX + g:PP_BX + g + 1], scale=1.0)
            yield
            a, ba_ = R['wt']()
            act(a[:, :nt], r[:, :nt], AF.Exp, [br, B_drv], [ba_], scale=drv[:, g:g + 1])
            s2, bs2 = R['wt']()
            act(s2[:, :nt], r[:, :nt], AF.Exp, [br, B_drv], [bs2], scale=drv[:, 64 + g:65 + g])
            tt(ig[:, :nt], ig[:, :nt], xcb[:, :nt], ALU.mult, [big, bxcb], [big])
            yield
            act(s2[:, :nt], s2[:, :nt], AF.Ln, [bs2, B_cst], [bs2], bias=one_c, scale=-1.0)
            act(s2[:, :nt], s2[:, :nt], AF.Exp, [bs2], [bs2], scale=0.5)
            yield
            tt(ig[:, :nt], ig[:, :nt], s2[:, :nt], ALU.mult, [big, bs2], [big])
            if not main:
                tt(ig[:, :nt], ig[:, :nt], mpre[:, :nt], ALU.mult, [big, B_mpre], [big])
            yield
            hs, bhs = R['wt']()
            S.op('dve', lambda e: e.tensor_tensor_scan(out=hs[:, :nt], data0=a[:, :nt], data1=ig[:, :nt],
                                                        initial=hcar[:, g:g + 1], op0=ALU.mult, op1=ALU.add),
                 [ba_, big, B_hcar], [bhs])
            yield
            cp('dve', hcar[:, g:g + 1], hs[:, nt - 1:nt], [bhs], [B_hcar])
            if main:
                for _ in range(RG_GELU_DELAY):
                    yield
                sy, bsy = load_slab(wcols(w_in, 2048 + g * 128))
                py, bpy = proj_fm2(sy, bsy, nt, R['ps_rg'])
                gy, bgy = R['wt']()
                act(gy[:, :nt], py[:, :nt], AF.Gelu, [bpy], [bgy])
                yield
                tt(YA[:, g, :nt], gy[:, :nt], hs[:, :nt], ALU.mult, [bgy, bhs], [(B_YA, g)])

        def hgrn2_head(g, nt, main, R=None):
            R = R or R0
            vbf, B_vbf = R['vbf']
            ket8, B_ket8 = R['ket8']
            Sall, B_Sall = R['Sall']
            sml, B_sml = R['sml']
            nch = nt // CH
            v3 = lambda t: t[:, :nt].rearrange("p (c t) -> p c t", t=CH)
            sv, bsv = load_slab(wcols(w_in, 8192 + g * 128))
            pvf, bpvf = proj_fm2(sv, bsv, nt, R['ps_hg'])
            vT, bvT = R['vT']
            cp('act', vT[:, :nt], pvf[:, :nt], [bpvf], [bvT])
            yield
            for c in range(nch):
                tr(pbb[0:64, c * 128:(c + 1) * 128], vT[:, c * CH:(c + 1) * CH], identb[:, :], [bvT, B_identb], [B_pbb])
            cp('dve', vbf[:, 0:nch, :], pbb[0:64, 0:nch * 128].rearrange("p (c k) -> p c k", k=128), [B_pbb], [B_vbf])
            yield
            sf, bsf = load_slab(wcols(w_in, 6144 + g * 128))
            pf, bpf = proj_fm2(sf, bsf, nt, R['ps_hg'])
            f, bf_ = htile()
            act(f[:, :nt], pf[:, :nt], AF.Sigmoid, [bpf], [bf_])
            yield
            lf, blf = htile()
            act(lf[:, :nt], f[:, :nt], AF.Ln, [bf_, B_drv], [blf], bias=drv[:, 16 + g:17 + g], scale=drv[:, 32 + g:33 + g])
            yield
            ts(f[:, :nt], f[:, :nt], drv[:, 48 + g:49 + g], drv[:, 32 + g:33 + g], ALU.mult, ALU.add, [bf_, B_drv], [bf_])
            bc, bbc = htile()
            S.op('dve', lambda e: e.tensor_tensor_scan(out=bc[:, :nt], data0=cst[:, C_RESET:C_RESET + nt], data1=lf[:, :nt],
                                                        initial=0.0, op0=ALU.mult, op1=ALU.add),
                 [B_cst, blf], [bbc])
            yield
            cp('dve', sml[:, 0:nch], v3(bc)[:, :, 31], [bbc], [B_sml])
            yield
            tt(sml[:, 8:8 + nch], v3(bc)[:, :, 63], sml[:, 0:nch], ALU.subtract, [bbc, B_sml], [B_sml])
            act(sml[:, 16:16 + nch], v3(bc)[:, :, 63], AF.Exp, [bbc], [B_sml])
            yield
            act(sml[:, 8:8 + nch], sml[:, 8:8 + nch], AF.Exp, [B_sml], [B_sml])
            act(sml[:, 24:24 + nch], sml[:, 0:nch], AF.Exp, [B_sml], [B_sml])
            tt(v3(bc), v3(bc), sml[:, 0:nch].unsqueeze(2).to_broadcast([128, nch, CH]), ALU.subtract, [bbc, B_sml], [bbc])
            yield
            act(lf[:, :nt], bc[:, :nt], AF.Exp, [bbc], [blf], scale=-1.0)
            act(bc[:, :nt], bc[:, :nt], AF.Exp, [bbc], [bbc])
            yield
            kd, bkd = R['kd']
            tt(kd[:, :nt], f[:, :nt], lf[:, :nt], ALU.mult, [bf_, blf], [bkd])
            yield
            ke, bke = R['ke']
            tt(v3(ke), v3(kd), sml[:, 8:8 + nch].unsqueeze(2).to_broadcast([128, nch, CH]), ALU.mult, [bkd, B_sml], [bke])
            yield
            if main:
                sq_, bsq = load_slab(wcols(w_in, 4096 + g * 128))
                pq, bpq = proj_fm2(sq_, bsq, nt, R['ps_hg'])
                qs, bqs = htile()
                act(qs[:, :nt], pq[:, :nt], AF.Silu, [bpq], [bqs])
                yield
                qd, bqd = wkb[3]
                tt(qd[:, :nt], qs[:, :nt], bc[:, :nt], ALU.mult, [bqs, bbc], [bqd])
                yield
                qe, bqe = wkb[4]
                tt(v3(qe), v3(qd), sml[:, 24:24 + nch].unsqueeze(2).to_broadcast([128, nch, CH]), ALU.mult, [bqd, B_sml], [bqe])
                yield
                so, bso = load_slab(wcols(w_in, 10240 + g * 128))
                pog, bpog = proj_fm2(so, bso, nt, R['ps_hg'])
                sgo, bsgo = htile()
                act(sgo[:, :nt], pog[:, :nt], AF.Silu, [bpog], [bsgo])
                yield
            po, bpo = pbanks[0]
            csl = lambda c: slice(c * CH, (c + 1) * CH)
            for c in range(nch):
                tr(pbb[0:64, c * 128:(c + 1) * 128], ke[:, csl(c)], identb[:, :], [bke, B_identb], [B_pbb])
            cp('act', ket8[:, 0:nch, :], pbb[0:64, 0:nch * 128].rearrange("p (c k) -> p c k", k=128), [B_pbb], [B_ket8])
            yield
            if main:
                psc, bpsc = R['ps_hg']()
                for c in range(nch):
                    mm(psc[0:64, c * 64:(c + 1) * 64], kd[:, csl(c)], qd[:, csl(c)], True, True, [bkd, bqd], [bpsc])
                tt(scm8[:, 0:nch, :], psc[0:64, 0:nch * 64].rearrange("p (c t) -> p c t", t=64),
                   maskTb[:, :].unsqueeze(1).to_broadcast([64, nch, 64]), ALU.mult, [bpsc, B_maskTb], [B_scm8])
                cp('act', Sbf8[:, 0, :], Sst[:, g, :], [(B_S, g)], [(B_Sbf8, 0)])
                yield
            def chain_step(c, psn_ap, bpsn):
                src, bsrc = (Sst[:, g, :], (B_S, g)) if c == 0 else (Sall[:, c, :], (B_Sall, c))
                dst, bdst = (Sst[:, g, :], (B_S, g)) if c == nch - 1 else (Sall[:, c + 1, :], (B_Sall, c + 1))
                stt(dst, src, sml[:, 16 + c:17 + c], psn_ap, ALU.mult, ALU.add, [bsrc, B_sml, bpsn], [bdst])

            if R['one_bank']:
                for c4 in range((nch + 3) // 4):
                    psn, bpsn = R['ps_hg']()
                    grp_l = []
                    for j in range(min(4, nch - c4 * 4)):
                        c = c4 * 4 + j
                        mm(psn[:, j * 128:(j + 1) * 128], ket8[:, c, :], vbf[:, c, :], True, True, [B_ket8, B_vbf], [bpsn])
                        grp_l.append((c, psn[:, j * 128:(j + 1) * 128], bpsn))
                    yield
                    for c, ap_, bb in grp_l:
                        chain_step(c, ap_, bb)
                        yield
            else:
                psn_l = []
                for c4 in range((nch + 3) // 4):
                    psn, bpsn = R['ps_hg']()
                    for j in range(min(4, nch - c4 * 4)):
                        c = c4 * 4 + j
                        mm(psn[:, j * 128:(j + 1) * 128], ket8[:, c, :], vbf[:, c, :], True, True, [B_ket8, B_vbf], [bpsn])
                        psn_l.append((psn[:, j * 128:(j + 1) * 128], bpsn))
                yield
                for c in range(nch):
                    chain_step(c, psn_l[c][0], psn_l[c][1])
                    yield
            if main:
                if nch > 1:
                    cp('act', Sbf8[:, 1:nch, :], Sall[:, 1:nch, :], [(B_Sall, c) for c in range(1, nch)],
                       [(B_Sbf8, c) for c in range(1, nch)])
                yield
                for c in range(nch):
                    mm(po[:, csl(c)], vbf[:, c, :], scm8[:, c, :], True, False, [B_vbf, B_scm8], [bpo])
                    mm(po[:, csl(c)], Sbf8[:, c, :], qe[:, csl(c)], False, True, [(B_Sbf8, c), bqe], [bpo])
                yield
                osq, bosq = htile()
                osqb = osq.bitcast(BF16)
                act(osqb[:, :nt], po[:, :nt], AF.Square, [bpo], [bosq])
                pm, bpm = R['ps_hg']()
                mm(pm[:, :nt], onesb[:, :], osqb[:, :nt], True, True, [B_onesb, bosq], [bpm])
                yield
                act(osq[:, :nt], pm[:, :nt], AF.Ln, [bpm, B_cst], [bosq], bias=eps_c, scale=1.0 / 128)
                yield
                act(osq[:, :nt], osq[:, :nt], AF.Exp, [bosq], [bosq], scale=-0.5)
                yield
                tt(osq[:, :nt], po[:, :nt], osq[:, :nt], ALU.mult, [bpo, bosq], [bosq])
                yield
                stt(YB[:, g, :nt], osq[:, :nt], pp[:, PP_NG + g:PP_NG + g + 1], sgo[:, :nt], ALU.mult, ALU.mult,
                    [bosq, B_pp, bsgo], [(B_YB, g)])

        def run_interleaved(*pairs):
            live = [[gen, st] for gen, st in pairs]
            t = 0
            while live:
                for item in list(live):
                    if item[1] > t and any(o[1] <= t for o in live):
                        continue
                    try:
                        next(item[0])
                    except StopIteration:
                        live.remove(item)
                t += 1

        def merge(nt):
            for m in range(NCH):
                spa, bspa = load_slab(wcols(w_pa, m * 128))
                pa_, bpa = ps()
                for c in range(NCH):
                    mm(pa_[:, :nt], spa[:, c, :], YA[:, c, :nt], c == 0, c == NCH - 1, [bspa, (B_YA, c)], [bpa])
                sza, bsza = load_slab(wcols(w_in, 12288 + m * 128))
                pza, bpza = proj_fm(sza, bsza, nt)
                za, bza = wtile()
                act(za[:, :nt], pza[:, :nt], AF.Sigmoid, [bpza], [bza])
                tt(za[:, :nt], pa_[:, :nt], za[:, :nt], ALU.mult, [bpa, bza], [bza])
                spb, bspb = load_slab(wcols(w_pb, m * 128))
                pb_, bpb = ps()
                for c in range(NCH):
                    mm(pb_[:, :nt], spb[:, c, :], YB[:, c, :nt], c == 0, c == NCH - 1, [bspb, (B_YB, c)], [bpb])
                szb, bszb = load_slab(wcols(w_in, 14336 + m * 128))
                pzb, bpzb = proj_fm(szb, bszb, nt)
                zb, bzb = wtile()
                act(zb[:, :nt], pzb[:, :nt], AF.Sigmoid, [bpzb], [bzb])
                tt(zb[:, :nt], pb_[:, :nt], zb[:, :nt], ALU.mult, [bpb, bzb], [bzb])
                tt(MIX[:, m, :nt], za[:, :nt], zb[:, :nt], ALU.add, [bza, bzb], [(B_MIX, m)])
            for m in range(NCH):
                swo, bswo = load_slab(wcols(w_out, m * 128))
                pd_, bpd = ps()
                for c in range(NCH):
                    mm(pd_[:, :nt], swo[:, c, :], MIX[:, c, :nt], c == 0, c == NCH - 1, [bswo, (B_MIX, c)], [bpd])
                tt(HT[:, m, :nt], HT[:, m, :nt], pd_[:, :nt], ALU.add, [(B_HT, m), bpd], [(B_HT, m)])

        out_events = []

        def final_out(t0, nt):
            rms_rows(nt, PP_GF, out_is_xt=False)
            for c in range(NCH):
                stt(HT[:, c, :nt], HT[:, c, :nt], pp[:, PP_GF + c:PP_GF + c + 1], rstd[:, :nt],
                    ALU.mult, ALU.mult, [(B_HT, c), B_pp, B_rstd], [(B_HT, c)])
            for ti in range(nt // 128):
                st, bst = xst[ti % 2]
                for c4 in range(4):
                    pt, pbuf = ps()
                    for j in range(4):
                        c = c4 * 4 + j
                        tr(pt[:, j * 128:(j + 1) * 128], HT[:, c, ti * 128:(ti + 1) * 128], ident, [(B_HT, c), B_cst], [pbuf])
                    cp('act', st[:, c4 * 512:(c4 + 1) * 512], pt[:, :], [pbuf], [bst] + XS_ALIAS[ti % 2])
                ev = S.dma('sp', y[t0 + ti * 128:t0 + (ti + 1) * 128, :], st[:, :], reads=[bst], writes=[B_y],
                           chan=('xst', ti % 2))
                out_events.append(ev)

        def vmax(out, in_, reads, writes):
            S.op('dve', lambda e: e.max(out=out, in_=in_), reads, writes)

        def vmaxidx(out, in_max, in_values, reads, writes):
            S.op('dve', lambda e: e.max_index(out=out, in_max=in_max, in_values=in_values), reads, writes)

        def vmrep(out, rep, vals, reads, writes):
            S.op('dve', lambda e: e.match_replace(out=out, in_to_replace=rep, in_values=vals, imm_value=-1e30), reads, writes)

        def vred(out, in_, reads, writes):
            S.op('dve', lambda e: e.tensor_reduce(out=out, in_=in_, axis=AX.X, op=ALU.add), reads, writes)

        def peer(nt):
            ntl = nt // 128
            rms_rows(nt, PP_G2)
            QT = MIX
            for m in range(NCH):
                sq_, bsq = load_slab(wcols(wq, m * 128))
                pq, bpq = proj_fm(sq_, bsq, nt)
                cp('act', QT[:, m, :nt], pq[:, :nt], [bpq], [(B_A3, m)])
            sk, bsk = load_slab(keysT.rearrange("m d k -> d m k"))
            sc = WK[:, 0:2048]
            sc_b = [wk[i][1] for i in range(0, 4)]
            sc2 = WK[:, 4 * WKW:4 * WKW + 2048]
            sc2_b = [wk[i][1] for i in range(4, 8)]
            cand = WK[:, 8 * WKW:8 * WKW + 2048]
            cand_b = [wk[i][1] for i in range(8, 12)]
            sc3 = sc.rearrange("p (m k) -> p m k", k=128)
            sc23 = sc2.rearrange("p (m k) -> p m k", k=128)
            cand4 = cand.rearrange("p (h a b) -> p h a b", a=16, b=16)
            cand3 = cand.rearrange("p (h c) -> p h c", c=256)
            scc3 = sc.rearrange("p (h c) -> p h c", c=256)
            tv4 = tv[:, :].rearrange("p (h q k) -> p h q k", q=2, k=16)
            tif4 = tif[:, :].rearrange("p (h q k) -> p h q k", q=2, k=16)
            bv3 = bv[:, :].rearrange("p (h k) -> p h k", k=16)
            g3 = pk[:, 5, :].rearrange("p (h k) -> p h k", k=16)
            ge3 = sc2.rearrange("p (x j) -> p x j", j=16)
            eq4 = sc2.rearrange("p (h k a) -> p h k a", k=16, a=16)
            iota16b = cst[:, C_IOTA16:C_IOTA16 + 16].unsqueeze(1).unsqueeze(1).to_broadcast([128, 8, 16, 16])
            thr16b = cst[:, C_THR16:C_THR16 + 16].unsqueeze(1).to_broadcast([128, 128, 16])
            for ti in range(ntl):
                tsl = slice(ti * 128, (ti + 1) * 128)
                for b4 in range(4):
                    pt, pbuf = ps()
                    for j in range(4):
                        m = b4 * 4 + j
                        mm(pt[:, j * 128:(j + 1) * 128], QT[:, m, tsl], sk[:, m, :], True, True, [(B_A3, m), bsk], [pbuf])
                    cp('act', sc[:, b4 * 512:(b4 + 1) * 512], pt[:, :], [pbuf], sc_b)
                t0_ = lambda m: tv[:, m * 16:m * 16 + 8]
                t1_ = lambda m: tv[:, m * 16 + 8:m * 16 + 16]
                for m in range(16):
                    vmax(t0_(m), sc3[:, m, :], sc_b, [(B_tv, m)])
                for m in range(16):
                    vmaxidx(tix[:, m * 16:m * 16 + 8], t0_(m), sc3[:, m, :], sc_b + [(B_tv, m)], [(B_tix, m)])
                for m in range(16):
                    vmrep(sc23[:, m, :], t0_(m), sc3[:, m, :], sc_b + [(B_tv, m)], sc2_b)
                for m in range(16):
                    vmax(t1_(m), sc23[:, m, :], sc2_b, [(B_tv, m)])
                for m in range(16):
                    vmaxidx(tix[:, m * 16 + 8:m * 16 + 16], t1_(m), sc23[:, m, :], sc2_b + [(B_tv, m)], [(B_tix, m)])
                cp('dve', tif[:, :], tix[:, :], [B_tix], [B_tif])
                tt(cand4, tv4[:, :, 0, :].unsqueeze(3).to_broadcast([128, 8, 16, 16]),
                   tv4[:, :, 1, :].unsqueeze(2).to_broadcast([128, 8, 16, 16]), ALU.add, [B_tv], cand_b)
                b0_ = lambda h: bv[:, h * 16:h * 16 + 8]
                b1_ = lambda h: bv[:, h * 16 + 8:h * 16 + 16]
                for h in range(8):
                    vmax(b0_(h), cand3[:, h, :], cand_b, [(B_bv, h)])
                for h in range(8):
                    vmaxidx(bpos[:, h * 16:h * 16 + 8], b0_(h), cand3[:, h, :], cand_b + [(B_bv, h)], [(B_bpos, h)])
                for h in range(8):
                    vmrep(scc3[:, h, :], b0_(h), cand3[:, h, :], cand_b + [(B_bv, h)], sc_b)
                for h in range(8):
                    vmax(b1_(h), scc3[:, h, :], sc_b, [(B_bv, h)])
                for h in range(8):
                    vmaxidx(bpos[:, h * 16 + 8:h * 16 + 16], b1_(h), scc3[:, h, :], sc_b + [(B_bv, h)], [(B_bpos, h)])
                tt(g3, bv3, bv3[:, :, 0:1].to_broadcast([128, 8, 16]), ALU.subtract, [B_bv], [(B_pk, 5)])
                act(pk[:, 5, :], pk[:, 5, :], AF.Exp, [(B_pk, 5)], [(B_pk, 5)])
                vred(hs8[:, 0:8], g3, [(B_pk, 5)], [B_hs8])
                S.op('dve', lambda e: e.reciprocal(out=hs8[:, 0:8], in_=hs8[:, 0:8]), [B_hs8], [B_hs8])
                tt(g3, g3, hs8[:, 0:8].unsqueeze(2).to_broadcast([128, 8, 16]), ALU.mult, [(B_pk, 5), B_hs8], [(B_pk, 5)])
                cp('dve', pk[:, 0, :], bpos[:, :], [B_bpos], [(B_pk, 0)])
                tt(ge3, pk[:, 0, :].unsqueeze(2).to_broadcast([128, 128, 16]), thr16b, ALU.is_ge, [(B_pk, 0), B_cst], sc2_b)
                vred(pk[:, 1, :], ge3, sc2_b, [(B_pk, 1)])
                stt(pk[:, 2, :], pk[:, 1, :], -16.0, pk[:, 0, :], ALU.mult, ALU.add, [(B_pk, 1), (B_pk, 0)], [(B_pk, 2)])
                for q in range(2):
                    ab3 = pk[:, 1 + q, :].rearrange("p (h k) -> p h k", k=16)
                    tt(eq4, ab3.unsqueeze(3).to_broadcast([128, 8, 16, 16]), iota16b, ALU.is_equal, [(B_pk, 1 + q), B_cst], sc2_b)
                    tt(eq4, eq4, tif4[:, :, q, :].unsqueeze(2).to_broadcast([128, 8, 16, 16]), ALU.mult, sc2_b + [B_tif], sc2_b)
                    vred(pk[:, 3 + q, :].rearrange("p (h k) -> p h k", k=16), eq4, sc2_b, [(B_pk, 3 + q)])
                pt, pbuf = ps()
                for q in range(3):
                    tr(pt[:, q * 128:(q + 1) * 128], pk[:, 3 + q, :], ident, [(B_pk, 3 + q), B_cst], [pbuf])
                cp('act', ijg[:, ti, :, :], pt[:, 0:384].rearrange("p (q n) -> p q n", n=128), [pbuf], [(B_ijg, ti)])
            GOIb = [AR1[:, k * 4096:(k + 1) * 4096].rearrange("p (n i) -> p n i", i=128) for k in range(2)]
            OJb = [AR2[:, k * 4096:(k + 1) * 4096].rearrange("p (n i) -> p n i", i=128) for k in range(2)]
            Wt_v = AR3[:, :].rearrange("p (g n c) -> p n g c", n=128, c=4)
            iota_b = cst[:, C_IOTA:C_IOTA + 128].unsqueeze(1).to_broadcast([128, 32, 128])
            S.op('dve', lambda e: e.memset(hs8[:, 8:9], 0.0), [], [B_YA, B_YB, B_hs8])
            sub = 0
            for ti in range(ntl):
                for sb_ in range(4):
                    k = sub % 2
                    sub += 1
                    ns = slice(sb_ * 32, (sb_ + 1) * 32)
                    iTs = ijg[:, ti, 0, ns].unsqueeze(2).to_broadcast([128, 32, 128])
                    jTs = ijg[:, ti, 1, ns].unsqueeze(2).to_broadcast([128, 32, 128])
                    gTs = ijg[:, ti, 2, ns].unsqueeze(2).to_broadcast([128, 32, 128])
                    bgoi = (B_YA, ('goi', k))
                    boj = (B_YB, ('oj', k))
                    tt(GOIb[k], iota_b, iTs, ALU.is_equal, [B_cst, (B_ijg, ti)], [bgoi])
                    tt(GOIb[k], GOIb[k], gTs, ALU.mult, [bgoi, (B_ijg, ti)], [bgoi])
                    tt(OJb[k], iota_b, jTs, ALU.is_equal, [B_cst, (B_ijg, ti)], [boj])
                    for n in range(32):
                        if n % 4 == 0:
                            pt, pbuf = ps()
                        mm(pt[:, (n % 4) * 128:(n % 4 + 1) * 128], GOIb[k][:, n, :], OJb[k][:, n, :], True, True, [bgoi, boj], [pbuf])
                        if n % 4 == 3:
                            ntok = sb_ * 32 + n
                            cp('act', Wt_v[:, ntok - 3:ntok + 1, :, :],
                               pt[:, :].rearrange("p (n g c) -> p n g c", g=32, c=4), [pbuf], [B_A3])
                S.dma('sp', wd_scr[ti, :, :], AR3[:, :], reads=[B_A3], writes=[(B_wd, ti)], chan='wdw')
            S.op('dve', lambda e: e.memset(hs8[:, 8:9], 0.0), [], [B_YA, B_YB, B_hs8])
            vring = [AR1[:, k * 2048:(k + 1) * 2048].rearrange("p (m d) -> p m d", d=128) for k in range(4)] + \
                    [AR2[:, k * 2048:(k + 1) * 2048].rearrange("p (m d) -> p m d", d=128) for k in range(4)]
            vring_b = [(B_YA, ('vs', k)) for k in range(4)] + [(B_YB, ('vs', k)) for k in range(4)]

            def stageA(grp):
                key = 'wg%d' % (grp % 2)
                wgt = AR3[:, (grp % 2) * 2048:(grp % 2) * 2048 + 2048].rearrange("p (t x) -> p t x", x=512)
                S.dma('sp', wgt[:, 0:ntl, :],
                      wd_scr.rearrange("t p (g x) -> p g t x", x=512)[:, grp, 0:ntl, :],
                      reads=[B_wd], writes=[(B_A3, key)], chan=('wg', grp % 2))
                wgv = AR3[:, (grp % 2) * 2048:(grp % 2) * 2048 + 2048].rearrange("p (tn c) -> p tn c", c=4)
                was, vs = [], []
                us = [load_slab(wcols(uT, (grp * 4 + j) * 128)) for j in range(4)]
                for j in range(4):
                    su, bsu = us[j]
                    pa_, bpa = proj_fm(su, bsu, nt)
                    k = (grp % 2) * 4 + j
                    ga = AR3[:, 4096 + k * 512:4096 + (k + 1) * 512]
                    bga = (B_A3, ('wa', k))
                    act(ga[:, :nt], pa_[:, :nt], AF.Gelu, [bpa], [bga])
                    tt(ga[:, :nt], ga[:, :nt], wgv[:, :nt, j], ALU.mult, [bga, (B_A3, key)], [bga])
                    was.append((ga, bga))
                for j in range(4):
                    c = grp * 4 + j
                    k = (grp % 2) * 4 + j
                    S.dma('pool', vring[k][:, :, :], vP[c * 128:(c + 1) * 128, :].rearrange("p (m d) -> p m d", d=128),
                          reads=[], writes=[vring_b[k]], chan=('vring', k))
                    vs.append((vring[k], vring_b[k]))
                return was, vs

            def stageB(was, vs):
                for m in range(NCH):
                    po_, bpo_ = ps()
                    for j in range(4):
                        mm(po_[:, :nt], vs[j][0][:, m, :], was[j][0][:, :nt], j == 0, j == 3, [vs[j][1], was[j][1]], [bpo_])
                    tt(HT[:, m, :nt], HT[:, m, :nt], po_[:, :nt], ALU.add, [(B_HT, m), bpo_], [(B_HT, m)])

            cur = stageA(0)
            for grp in range(32):
                nxt = stageA(grp + 1) if grp + 1 < 32 else None
                stageB(*cur)
                cur = nxt

        t0 = 0
        for nt in pre_blocks:
            S.dma('sp', mpre[:, :nt], maskpre[:, t0:t0 + nt], writes=[B_mpre], chan='mpre')
            load_block(xpre, t0, nt)
            rms_rows(nt, PP_G1)
            slab_mode[0] = 'pre'
            if PRE_PAIR:
                for g in range(0, 16, 2):
                    run_interleaved((rglru_head(g, nt, False, t0, R0pre), 0), (hgrn2_head(g, nt, False, R0pre), HG_OFFSET),
                                    (rglru_head(g + 1, nt, False, t0, R1), PAIR_OFFSET),
                                    (hgrn2_head(g + 1, nt, False, R1), PAIR_OFFSET + HG_OFFSET))
            else:
                for g in range(16):
                    run_interleaved((rglru_head(g, nt, False, t0), 0), (hgrn2_head(g, nt, False), HG_OFFSET))
            slab_mode[0] = 'base'
            t0 += nt
        if PRE_PAIR:
            S.op('dve', lambda e: e.memset(hs8[:, 10:11], 0.0), R1_ALLBUFS, [B_YA, B_YB, B_hs8])
        t0 = 0
        for nt in main_blocks:
            load_block(xmain, t0, nt)
            rms_rows(nt, PP_G1)
            S.op('dve', lambda e: e.memset(hs8[:, 9:10], 0.0), [], [B_A3, B_hs8])
            slab_mode[0] = 'heads'
            for g in range(16):
                run_interleaved((rglru_head(g, nt, True, t0), 0), (hgrn2_head(g, nt, True), HG_OFFSET))
            slab_mode[0] = 'base'
            merge(nt)
            if do_peer:
                peer(nt)
            final_out(t0, nt)
            t0 += nt
        S.final_wait('sp', out_events)
        if dbg:
            print(act_log)
        S.emit()
    return nc


def _pp_layout(v):
    return np.ascontiguousarray(np.asarray(v, np.float32).reshape(16, 128).T)


def make_shared(inp):
    pp = np.zeros((128, NPP), np.float32)
    pp[:, PP_G1:PP_G1 + 16] = _pp_layout(inp['ln1_g'][0])
    pp[:, PP_G2:PP_G2 + 16] = _pp_layout(inp['ln2_g'][0])
    pp[:, PP_GF:PP_GF + 16] = _pp_layout(inp['final_g'])
    cw = np.asarray(inp['conv_w'][0], np.float32)
    pp[:, PP_CW:PP_CW + 64] = np.ascontiguousarray(cw.reshape(4, 16, 128).transpose(2, 1, 0)).reshape(128, 64)
    pp[:, PP_CB:PP_CB + 16] = _pp_layout(inp['conv_b'][0])
    pp[:, PP_BA:PP_BA + 16] = _pp_layout(inp['rg_ba'][0])
    pp[:, PP_BX:PP_BX + 16] = _pp_layout(inp['rg_bx'][0])
    pp[:, PP_LAM:PP_LAM + 16] = _pp_layout(inp['rg_lambda'][0])
    pp[:, PP_L0:PP_L0 + 16] = _pp_layout(inp['hg_lb_logits'][0])
    pp[:, PP_L1:PP_L1 + 16] = _pp_layout(inp['hg_lb_logits'][1])
    pp[:, PP_NG:PP_NG + 16] = _pp_layout(inp['hg_norm_g'][0])
    cs = np.zeros((128, NCONST), np.float32)
    cs[:, C_ID:C_ID + 128] = np.eye(128, dtype=np.float32)
    s = np.arange(64)
    cs[0:64, C_MASKT:C_MASKT + 64] = (s[:, None] <= s[None, :]).astype(np.float32)
    rm = np.ones(512, np.float32)
    rm[::64] = 0.0
    cs[:, C_RESET:C_RESET + 512] = rm[None, :]
    cs[:, C_IOTA:C_IOTA + 128] = np.arange(128, dtype=np.float32)[None, :]
    cs[:, C_ONES:C_ONES + 128] = 1.0
    cs[:, C_EPS] = EPS
    cs[:, C_ONE] = 1.0
    cs[:, C_IOTA16:C_IOTA16 + 16] = np.arange(16, dtype=np.float32)[None, :]
    thr = (np.arange(16, dtype=np.float32) + 1.0) * 16.0
    thr[15] = 1e9
    cs[:, C_THR16:C_THR16 + 16] = thr[None, :]
    keys = np.asarray(inp['peer_keys'][0], np.float32)
    keysT = np.ascontiguousarray(keys.reshape(16, 128, 128).transpose(0, 2, 1))
    u = np.asarray(inp['peer_u'][0], np.float32)
    v = np.asarray(inp['peer_v'][0], np.float32)
    uT = np.ascontiguousarray(u.reshape(128, 128, 16, 128).transpose(1, 3, 2, 0)).reshape(128, 128, D)
    vP = np.ascontiguousarray(v.reshape(128, 128, D).transpose(1, 0, 2).reshape(NEXP, D))
    def slabify(w):
        w = np.asarray(w, np.float32)
        ns = w.shape[1] // 128
        return np.ascontiguousarray(w.reshape(16, 128, ns, 128).transpose(2, 1, 0, 3)).reshape(ns, 128, D)

    return {
        'w_in': slabify(inp['w_in'][0]),
        'w_pa': slabify(inp['w_pa'][0]),
        'w_pb': slabify(inp['w_pb'][0]),
        'w_out': slabify(inp['w_out'][0]),
        'wq': slabify(inp['peer_wq'][0]),
        'rg_wa': np.ascontiguousarray(inp['rg_wa'][0], np.float32),
        'rg_wx': np.ascontiguousarray(inp['rg_wx'][0], np.float32),
        'pp': pp, 'consts': cs, 'keysT': keysT, 'uT': uT, 'vP': vP,
    }


HG_OFFSET = 4
WARM_K = 0
PRE_PAIR = 1
PAIR_OFFSET = 2
RG_GELU_DELAY = 3
PRE_BLOCKS = [512, 512, 512, 512, 128]
MAIN_BLOCKS = [512, 512, 512, 512]


def kernel(**inputs):
    x = np.asarray(inputs['x'], np.float32)
    meta = np.asarray(inputs['meta'], np.float32)
    B, T, _ = x.shape
    half = T // 2
    shared = make_shared(inputs)
    npre = sum(PRE_BLOCKS)
    in_maps = []
    for b in range(B):
        for s in range(2):
            xpre = np.zeros((npre, D), np.float32)
            mask = np.zeros((128, npre), np.float32)
            if s == 0:
                xpre[npre - 16:] = meta
                mask[:, npre - 16:] = 1.0
            else:
                xpre[npre - half - 16:npre - half] = meta
                xpre[npre - half:] = x[b, :half]
                mask[:, npre - half - 16:] = 1.0
            m = dict(shared)
            m['xpre'] = xpre
            m['maskpre'] = mask
            m['xmain'] = np.ascontiguousarray(x[b, s * half:(s + 1) * half])
            in_maps.append(m)
    nc = build_program(PRE_BLOCKS, MAIN_BLOCKS, do_peer=True)
    res = run_bass_kernel_spmd(nc, in_maps, core_ids=list(range(8)))
    out = np.zeros((B, T, D), np.float32)
    i = 0
    for b in range(B):
        for s in range(2):
            out[b, s * half:(s + 1) * half] = res.results[i]['y']
            i += 1
    return out
```
